# Optimizing a Trainium2 kernel written in Bass

```python
import math
import jax, jax.numpy as jnp
from jax import lax
import numpy as np

D_MODEL = 2048
BATCH = 8
SEQ = 2048
DEPTH = 2

HEAD_DIM = 128
N_HEADS_DIFF = 8
DIFF_MAP_DIM = HEAD_DIM // 2
N_HEADS_SB = 8
N_HEADS_MOBA = 8
MIX_W = 8 * HEAD_DIM
N_BRANCH = 3
MOBA_BLOCK = 256
MOBA_TOPK = 3
Q_BLOCK = 128
MOBA_Q_CHUNK = 16
D_FF = -(-8 * D_MODEL // (3 * 256)) * 256
RMS_EPS = 1e-6
COL_WIDTHS = [N_HEADS_DIFF * 2 * DIFF_MAP_DIM, N_HEADS_DIFF * 2 * DIFF_MAP_DIM, N_HEADS_DIFF * HEAD_DIM,
              N_HEADS_SB * HEAD_DIM, N_HEADS_SB * HEAD_DIM, N_HEADS_SB * HEAD_DIM,
              N_HEADS_MOBA * HEAD_DIM, N_HEADS_MOBA * HEAD_DIM, N_HEADS_MOBA * HEAD_DIM,
              N_BRANCH * D_MODEL]
N_COLS = sum(COL_WIDTHS)
COL_OFFSETS = [int(o) for o in np.cumsum(COL_WIDTHS)[:-1]]
N_ALIBI = N_HEADS_DIFF + N_HEADS_MOBA
ALIBI_ALL = 2.0 ** (-8.0 * (np.arange(N_ALIBI) + 1) / N_ALIBI)
ALIBI_DIFF = ALIBI_ALL[0::2].astype(np.float32)
ALIBI_MOBA = ALIBI_ALL[1::2].astype(np.float32)

kernel_name = 'hybrid_diff_stickbreak_moba_gated'


def rms_norm(x, g):
    xf = x.astype(jnp.float32)
    y = xf * lax.rsqrt(jnp.mean(xf * xf, axis=-1, keepdims=True) + RMS_EPS)
    return (y * g.astype(jnp.float32)).astype(x.dtype)


def _split_heads(t, n):
    b, s, _ = t.shape
    return t.reshape(b, s, n, -1).transpose(0, 2, 1, 3)


def _merge_heads(o):
    b, h, s, d = o.shape
    return o.transpose(0, 2, 1, 3).reshape(b, s, h * d)


def _blocks(q, size):
    b, h, s = q.shape[:3]
    rest = q.shape[3:]
    qb = q.reshape((b, h, s // size, size) + rest)
    return jnp.moveaxis(qb, 2, 0)


def _unblocks(o):
    nq, b, h, size, d = o.shape
    return jnp.moveaxis(o, 0, 2).reshape(b, h, nq * size, d)


def diff_attention(q, k, v, lam, subln_w, lambda_init):
    b, h, s, _, dm = q.shape
    scale = dm ** -0.5
    kpos = jnp.arange(s)
    slopes = jnp.asarray(ALIBI_DIFF)
    nq = s // Q_BLOCK

    def block(args):
        qi, i = args
        t = i * Q_BLOCK + jnp.arange(Q_BLOCK)
        sc = jnp.einsum('bhqmd,bhkmd->bhmqk', qi, k).astype(jnp.float32) * scale
        dist = (t[:, None] - kpos[None, :]).astype(jnp.float32)
        sc = sc - (slopes[:, None, None] * dist[None])[None, :, None]
        sc = jnp.where(kpos[None, :] <= t[:, None], sc, -jnp.inf)
        p = jax.nn.softmax(sc, axis=-1)
        a = p[:, :, 0] - lam * p[:, :, 1]
        return jnp.einsum('bhqk,bhkd->bhqd', a.astype(v.dtype), v)

    o = _unblocks(lax.map(block, (_blocks(q, Q_BLOCK), jnp.arange(nq))))
    o = rms_norm(o, subln_w).astype(jnp.float32) * (1.0 - lambda_init)
    return o.astype(v.dtype)


def stick_breaking_attention(q, k, v):
    b, h, s, d = q.shape
    scale = d ** -0.5
    kpos = jnp.arange(s)
    nq = s // Q_BLOCK

    def block(args):
        qi, i = args
        t = i * Q_BLOCK + jnp.arange(Q_BLOCK)
        z = jnp.einsum('bhqd,bhkd->bhqk', qi, k).astype(jnp.float32) * scale
        strict = kpos[None, :] < t[:, None]
        log_fail = jnp.where(strict, jax.nn.log_sigmoid(-z), 0.0)
        suffix = lax.cumsum(log_fail, axis=log_fail.ndim - 1, reverse=True) - log_fail
        w = jnp.where(strict, jnp.exp(jax.nn.log_sigmoid(z) + suffix), 0.0)
        return jnp.einsum('bhqk,bhkd->bhqd', w.astype(v.dtype), v)

    return _unblocks(lax.map(block, (_blocks(q, Q_BLOCK), jnp.arange(nq))))


def moba_attention(q, k, v):
    b, h, s, d = q.shape
    scale = d ** -0.5
    slopes = jnp.asarray(ALIBI_MOBA)
    nb = -(-s // MOBA_BLOCK)
    pad = nb * MOBA_BLOCK - s
    kp = jnp.pad(k, ((0, 0), (0, 0), (0, pad), (0, 0))).reshape(b, h, nb, MOBA_BLOCK, d)
    vp = jnp.pad(v, ((0, 0), (0, 0), (0, pad), (0, 0))).reshape(b, h, nb, MOBA_BLOCK, d)
    kmean = jnp.mean(kp.astype(jnp.float32), axis=3)
    k_sel = max(1, min(MOBA_TOPK, nb - 1))
    bi = jnp.arange(b)[:, None, None, None]
    hi = jnp.arange(h)[None, :, None, None]
    blk = jnp.arange(MOBA_BLOCK)
    nc = s // MOBA_Q_CHUNK

    def chunk(args):
        qi, c = args
        t = c * MOBA_Q_CHUNK + jnp.arange(MOBA_Q_CHUNK)
        own = (c * MOBA_Q_CHUNK) // MOBA_BLOCK
        gate = jnp.einsum('bhqd,bhnd->bhqn', qi.astype(jnp.float32), kmean)
        gate = jnp.where(jnp.arange(nb) < own, gate, -jnp.inf)
        _, idx = lax.top_k(gate, k_sel)
        valid = idx < own
        ksel = kp[bi, hi, idx]
        vsel = vp[bi, hi, idx]
        pos_sel = idx[..., None] * MOBA_BLOCK + blk
        s_sel = jnp.einsum('bhqd,bhqkpd->bhqkp', qi, ksel).astype(jnp.float32) * scale
        s_sel = s_sel - slopes[None, :, None, None, None] * (t[None, None, :, None, None] - pos_sel).astype(jnp.float32)
        s_sel = jnp.where(valid[..., None], s_sel, -jnp.inf)
        kown = lax.dynamic_index_in_dim(kp, own, axis=2, keepdims=False)
        vown = lax.dynamic_index_in_dim(vp, own, axis=2, keepdims=False)
        pos_own = own * MOBA_BLOCK + blk
        s_own = jnp.einsum('bhqd,bhpd->bhqp', qi, kown).astype(jnp.float32) * scale
        s_own = s_own - slopes[:, None, None] * (t[:, None] - pos_own[None, :]).astype(jnp.float32)
        s_own = jnp.where(pos_own[None, :] <= t[:, None], s_own, -jnp.inf)
        s_all = jnp.concatenate([s_sel.reshape(b, h, MOBA_Q_CHUNK, k_sel * MOBA_BLOCK), s_own], axis=-1)
        p = jax.nn.softmax(s_all, axis=-1).astype(v.dtype)
        p_sel = p[..., :k_sel * MOBA_BLOCK].reshape(b, h, MOBA_Q_CHUNK, k_sel, MOBA_BLOCK)
        p_own = p[..., k_sel * MOBA_BLOCK:]
        return (jnp.einsum('bhqkp,bhqkpd->bhqd', p_sel, vsel)
                + jnp.einsum('bhqp,bhpd->bhqd', p_own, vown))

    return _unblocks(lax.map(chunk, (_blocks(q, MOBA_Q_CHUNK), jnp.arange(nc))))


def hybrid_mixer(h, w_in, b_gate, lam_q1, lam_k1, lam_q2, lam_k2, subln_w, w_branch, w_out, lambda_init):
    b, s, _ = h.shape
    proj = h @ w_in
    qa, ka, va, qb, kb, vb, qc, kc, vc, g = jnp.split(proj, COL_OFFSETS, axis=-1)
    qa = _split_heads(qa, N_HEADS_DIFF).reshape(b, N_HEADS_DIFF, s, 2, DIFF_MAP_DIM)
    ka = _split_heads(ka, N_HEADS_DIFF).reshape(b, N_HEADS_DIFF, s, 2, DIFF_MAP_DIM)
    lam = (jnp.exp(jnp.sum(lam_q1.astype(jnp.float32) * lam_k1.astype(jnp.float32)))
           - jnp.exp(jnp.sum(lam_q2.astype(jnp.float32) * lam_k2.astype(jnp.float32))) + lambda_init)
    o_a = diff_attention(qa, ka, _split_heads(va, N_HEADS_DIFF), lam, subln_w, lambda_init)
    o_b = stick_breaking_attention(_split_heads(qb, N_HEADS_SB), _split_heads(kb, N_HEADS_SB), _split_heads(vb, N_HEADS_SB))
    o_c = moba_attention(_split_heads(qc, N_HEADS_MOBA), _split_heads(kc, N_HEADS_MOBA), _split_heads(vc, N_HEADS_MOBA))
    br = jnp.stack([_merge_heads(o_a), _merge_heads(o_b), _merge_heads(o_c)], axis=2)
    z = jnp.einsum('bsnm,nmd->bsnd', br, w_branch)
    gates = jax.nn.sigmoid((g + b_gate).reshape(b, s, N_BRANCH, D_MODEL))
    merged = jnp.sum(gates * z, axis=2)
    return merged @ w_out


def swiglu(h, w_gate, w_up, w_down):
    return (jax.nn.silu(h @ w_gate) * (h @ w_up)) @ w_down


def setup_inputs(seed: int = 0) -> dict:
    key = jax.random.key(seed)
    ks = jax.random.split(key, 16)
    f32 = jnp.float32
    nrm = lambda k, shape, sc: jax.random.normal(k, shape, f32) * sc
    out_sc = (2.0 * DEPTH) ** -0.5
    return {
        'x': nrm(ks[0], (BATCH, SEQ, D_MODEL), 1.0),
        'norm_mix_g': 1.0 + nrm(ks[1], (DEPTH, D_MODEL), 0.02),
        'norm_ffn_g': 1.0 + nrm(ks[2], (DEPTH, D_MODEL), 0.02),
        'w_in': nrm(ks[3], (DEPTH, D_MODEL, N_COLS), D_MODEL ** -0.5),
        'b_gate': nrm(ks[4], (DEPTH, N_BRANCH * D_MODEL), 0.01),
        'lam_q1': nrm(ks[5], (DEPTH, DIFF_MAP_DIM), 0.1),
        'lam_k1': nrm(ks[6], (DEPTH, DIFF_MAP_DIM), 0.1),
        'lam_q2': nrm(ks[7], (DEPTH, DIFF_MAP_DIM), 0.1),
        'lam_k2': nrm(ks[8], (DEPTH, DIFF_MAP_DIM), 0.1),
        'subln_w': 1.0 + nrm(ks[9], (DEPTH, HEAD_DIM), 0.02),
        'w_branch': nrm(ks[10], (DEPTH, N_BRANCH, MIX_W, D_MODEL), MIX_W ** -0.5),
        'w_out': nrm(ks[11], (DEPTH, D_MODEL, D_MODEL), D_MODEL ** -0.5 * out_sc),
        'w_ffn_gate': nrm(ks[12], (DEPTH, D_MODEL, D_FF), D_MODEL ** -0.5),
        'w_ffn_up': nrm(ks[13], (DEPTH, D_MODEL, D_FF), D_MODEL ** -0.5),
        'w_ffn_down': nrm(ks[14], (DEPTH, D_FF, D_MODEL), D_FF ** -0.5 * out_sc),
        'final_norm_g': 1.0 + nrm(ks[15], (D_MODEL,), 0.02),
    }


def reference(x, norm_mix_g, norm_ffn_g, w_in, b_gate, lam_q1, lam_k1, lam_q2, lam_k2, subln_w,
              w_branch, w_out, w_ffn_gate, w_ffn_up, w_ffn_down, final_norm_g):
    for l in range(DEPTH):
        lambda_init = 0.8 - 0.6 * math.exp(-0.3 * l)
        h = rms_norm(x, norm_mix_g[l])
        x = x + hybrid_mixer(h, w_in[l], b_gate[l], lam_q1[l], lam_k1[l], lam_q2[l], lam_k2[l],
                             subln_w[l], w_branch[l], w_out[l], lambda_init)
        h = rms_norm(x, norm_ffn_g[l])
        x = x + swiglu(h, w_ffn_gate[l], w_ffn_up[l], w_ffn_down[l])
    return rms_norm(x, final_norm_g)
```

```python
import math
import numpy as np
import ml_dtypes
import concourse.bass as bass
import concourse.mybir as mybir
from concourse.bass_utils import run_bass_kernel_spmd

F32 = mybir.dt.float32
BF16 = mybir.dt.bfloat16
AF = mybir.ActivationFunctionType
ALU = mybir.AluOpType
AX = mybir.AxisListType

D = 2048
S = 2048
DEPTH = 2
NCOLS = 15360
DFF = 5632
EPS = 1e-6
NEG = -30000.0
N_ALIBI = 16
ALIBI_ALL = 2.0 ** (-8.0 * (np.arange(N_ALIBI) + 1) / N_ALIBI)
ALIBI_DIFF = ALIBI_ALL[0::2].astype(np.float32)
ALIBI_MOBA = ALIBI_ALL[1::2].astype(np.float32)

SB_BASE = 16512
SB_TOP = 229344


class Res:
    __slots__ = ("name", "w", "r")

    def __init__(self, name=""):
        self.name = name
        self.w = None
        self.r = []


class Stream:
    def __init__(self, name):
        self.name = name
        self.items = []
        self.count = 0
        self.sem = None
        self.waited = {}
        self.ring = []
        self.ring_pos = 0


class Ctx:
    COMPUTE = ("pe", "act", "dve", "pool")

    def __init__(self, nc):
        self.nc = nc
        self.st = {n: Stream(n) for n in ("pe", "act", "dve", "pool", "sp")}
        for n in self.COMPUTE:
            self.st[n].sem = nc.alloc_semaphore("c_" + n)
        for n, k in (("sp", 4), ("pool", 4), ("act", 2)):
            for i in range(k):
                self.st[n].ring.append([nc.alloc_semaphore("d_%s%d" % (n, i)), 0, None])
        self.sb_off = SB_BASE
        self.last_barrier = []
        self.nalloc = 0
        self.bank = 0

    def sb(self, shape, dtype, name="t"):
        nbytes = int(np.prod(shape[1:])) * (4 if dtype == F32 else 2)
        nbytes = (nbytes + 31) // 32 * 32
        assert self.sb_off + nbytes <= SB_TOP, ("sbuf overflow", name, self.sb_off, nbytes)
        self.nalloc += 1
        t = self.nc.alloc_sbuf_tensor_at("%s_%d" % (name, self.nalloc), list(shape), dtype, offset=self.sb_off)
        self.sb_off += nbytes
        return t

    def mark(self):
        return self.sb_off

    def release(self, m):
        self.sb_off = m

    def _need(self, st, tok, waits):
        if tok is None:
            return
        kind, key, val, sem = tok
        if kind == "c" and key == "pe" and st.name == "pe":
            return
        k = (kind, key)
        if st.waited.get(k, 0) >= val:
            return
        st.waited[k] = val
        waits.append((sem, val))

    def _collect(self, st, reads, writes):
        waits = []
        for r in reads:
            self._need(st, r.w, waits)
        for w in writes:
            self._need(st, w.w, waits)
            for t in w.r:
                self._need(st, t, waits)
        return waits

    def _update(self, tok, reads, writes):
        for r in reads:
            r.r.append(tok)
        for w in writes:
            w.w = tok
            w.r = []

    def op(self, eng, fn, reads=(), writes=()):
        st = self.st[eng]
        waits = self._collect(st, reads, writes)
        st.count += 1
        tok = ("c", eng, st.count, st.sem)
        st.items.append((waits, fn, (st.sem, 1)))
        self._update(tok, reads, writes)
        return tok

    def dma(self, q, out, in_, reads=(), writes=(), phase_local=False):
        st = self.st[q]
        waits = self._collect(st, reads, writes)
        if phase_local:
            for t in self.last_barrier:
                if not (t[0] == "c" and t[1] == q):
                    self._need(st, t, waits)
        slot = st.ring[st.ring_pos % len(st.ring)]
        st.ring_pos += 1
        self._need(st, slot[2], waits)
        slot[1] += 16
        tok = ("d", slot[0].num, slot[1], slot[0])
        slot[2] = tok
        st.items.append((waits, (lambda e, o=out, i=in_: e.dma_start(out=o, in_=i)), (slot[0], 16)))
        self._update(tok, reads, writes)
        return tok

    def barrier(self, engines=("pe", "act", "dve", "sp")):
        toks = []
        for n in self.COMPUTE:
            s = self.st[n]
            if s.count:
                toks.append(("c", n, s.count, s.sem))
        for n in ("sp", "pool", "act"):
            for slot in self.st[n].ring:
                if slot[2] is not None:
                    toks.append(slot[2])
        self.last_barrier = toks
        for n in engines:
            st = self.st[n]
            waits = []
            for t in toks:
                if t[0] == "c" and t[1] == n:
                    continue
                self._need(st, t, waits)
            if waits:
                st.items.append((waits, None, None))

    def next_bank(self):
        b = self.bank
        self.bank = (self.bank + 1) % 8
        return b

    def emit(self):
        nc = self.nc
        st = self.st

        def run(s, e):
            for waits, fn, inc in s.items:
                for sem, val in waits:
                    e.wait_ge(sem, val)
                if fn is not None:
                    ins = fn(e)
                    ins.then_inc(inc[0], inc[1])

        with nc.Block() as block:
            @block.tensor
            def _(e):
                run(st["pe"], e)

            @block.scalar
            def _(e):
                run(st["act"], e)

            @block.vector
            def _(e):
                run(st["dve"], e)

            @block.gpsimd
            def _(e):
                run(st["pool"], e)

            @block.sync
            def _(e):
                run(st["sp"], e)


class Prog:
    def __init__(self, n_layers=DEPTH, stop_after=None, taps=(), skip=()):
        self.skip = set(skip)
        self.n_layers = n_layers
        self.stop_after = stop_after
        self.taps = set(taps)
        nc = self.nc = bass.Bass("TRN2", target_bir_lowering=False)
        self.cx = Ctx(nc)
        ein = lambda name, shape, dt=F32: nc.dram_tensor(name, list(shape), dt, kind="ExternalInput").ap()
        self.xT = ein("xT", [D, S])
        self.w_in = ein("w_in", [DEPTH, D, NCOLS])
        self.w_branch = ein("w_branch", [DEPTH, 3, 1024, D])
        self.w_out = ein("w_out", [DEPTH, D, D])
        self.w_gate = ein("w_ffn_gate", [DEPTH, D, DFF])
        self.w_up = ein("w_ffn_up", [DEPTH, D, DFF])
        self.w_down = ein("w_ffn_down", [DEPTH, DFF, D])
        self.g_mix = ein("g_mix", [DEPTH, 128, 16])
        self.g_ffn = ein("g_ffn", [DEPTH, 128, 16])
        self.g_fin = ein("g_fin", [128, 16])
        self.b_gate = ein("b_gate", [DEPTH, 128, 48])
        self.lamv = ein("lamv", [DEPTH, 4, 64])
        self.subln = ein("subln", [DEPTH, 128, 1])
        self.c_bf = ein("c_bf", [128, CB_COLS], BF16)
        self.c_f32 = ein("c_f32", [128, CF_COLS])
        self.c_rows = ein("c_rows", [NROWS, S], BF16)
        self.outT = nc.dram_tensor("outT", [D, S], F32, kind="ExternalOutput").ap()

        def scratch(name, shape, dt):
            kind = "ExternalOutput" if name in self.taps else "Internal"
            return nc.dram_tensor(name, list(shape), dt, kind=kind).ap()
        self.xres = scratch("xres", [D, S], F32)
        self.projT = scratch("projT", [9216, S], BF16)
        self.vtok = scratch("vtok", [3, S, 1024], BF16)
        self.gateT = scratch("gateT", [6144, S], BF16)
        self.oT = scratch("oT", [3, 1024, S], BF16)
        self.mergedT = scratch("mergedT", [D, S], BF16)
        self.actT = scratch("actT", [DFF, S], BF16)
        self.hdbg = scratch("hdbg", [D, S], BF16)

        cx = self.cx
        self.cb = cx.sb([128, CB_COLS], BF16, "cb")
        self.cf = cx.sb([128, CF_COLS], F32, "cf")
        self.cres = Res("consts")
        cx.dma("sp", self.cb[:], self.c_bf[:, :], writes=[self.cres])
        cx.dma("sp", self.cf[:], self.c_f32[:, :], writes=[self.cres])
        self.NSLOT = 2
        self.wslot = [cx.sb([128, 12288], BF16, "wslot") for _ in range(self.NSLOT)]
        self.wres = [Res("wslot%d" % i) for i in range(self.NSLOT)]
        self.wpos = 0
        self.ps = [nc.alloc_psum_tensor("ps%d" % i, [128, 512], F32) for i in range(8)]
        self.pres = [Res("ps%d" % i) for i in range(8)]
        self.xres_r = [Res("xres%d" % i) for i in range(16)]
        self.persist_mark = cx.mark()

        self.build()
        cx.barrier(engines=("pe", "act", "dve", "pool", "sp"))
        cx.emit()

    def ones(self):
        return self.cb[:, CB_ONES:CB_ONES + 128]

    def next_slot(self):
        s = self.wpos % self.NSLOT
        self.wpos += 1
        return s

    def bank(self):
        b = self.cx.next_bank()
        return self.ps[b], self.pres[b]

    def build(self):
        xsrc = self.xT
        for l in range(self.n_layers):
            steps = [
                ("proj", lambda: self.phase_proj(l, xsrc)),
                ("attn_a", lambda: self.phase_attn_a(l)),
                ("attn_b", lambda: self.phase_attn_b(l)),
                ("attn_c", lambda: self.phase_attn_c(l)),
                ("merge", lambda: self.phase_merge(l)),
                ("wout", lambda: self.residual_gemm(xsrc, self.w_out[l], self.mergedT, 16)),
                ("ffn_up", lambda: self.phase_ffn_up(l)),
            ]
            for name, fn in steps:
                if name in self.skip:
                    continue
                fn()
                if self.stop_after == (name, l):
                    return
            self.residual_gemm(self.xres, self.w_down[l], self.actT, 11, npass=4)
            if self.stop_after == ("ffn_down", l):
                return
            xsrc = self.xres
        self.phase_final(xsrc)

    def rmsnorm_to_sbuf(self, xsrc, g_ap, hT, hres, out_dram=None):
        cx = self.cx
        m = cx.mark()
        gt = cx.sb([128, 16], F32, "g")
        gres = Res("g")
        cx.dma("sp", gt[:], g_ap, writes=[gres])
        xin = [cx.sb([128, S], F32, "xin") for _ in range(2)]
        xin_r = [Res("xin") for _ in range(2)]
        sq = [cx.sb([128, S], BF16, "sq") for _ in range(2)]
        sq_r = [Res("sq") for _ in range(2)]
        rstd = cx.sb([128, S], F32, "rstd")
        rstd_r = Res("rstd")
        banks = [self.bank() for _ in range(4)]
        ones = self.ones()
        for kc in range(16):
            b = kc % 2
            cx.dma("sp", xin[b][:], xsrc[kc * 128:(kc + 1) * 128, :], reads=[self.xres_r[kc]], writes=[xin_r[b]])
            cx.op("act", lambda e, b=b: e.activation(out=sq[b][:], in_=xin[b][:], func=AF.Square),
                  reads=[xin_r[b]], writes=[sq_r[b]])

            def mm(e, b=b, kc=kc):
                ins = None
                for tg in range(4):
                    ins = e.matmul(banks[tg][0][:], ones, sq[b][:, tg * 512:(tg + 1) * 512],
                                   start=(kc == 0), stop=(kc == 15))
                return ins
            cx.op("pe", mm, reads=[sq_r[b], self.cres], writes=[bk[1] for bk in banks])
            if out_dram is None:
                cx.op("dve", lambda e, b=b, kc=kc: e.tensor_copy(out=hT[:, kc, :], in_=xin[b][:]),
                      reads=[xin_r[b]], writes=[hres[kc]])
        for tg in range(4):
            cx.op("act", lambda e, tg=tg: e.activation(out=rstd[:, tg * 512:(tg + 1) * 512], in_=banks[tg][0][:],
                                                        func=AF.Ln, bias=self.cf[:, CF_EPS:CF_EPS + 1], scale=1.0 / D),
                  reads=[banks[tg][1], self.cres], writes=[rstd_r])
        cx.op("act", lambda e: e.activation(out=rstd[:], in_=rstd[:], func=AF.Exp, scale=-0.5), reads=[rstd_r], writes=[rstd_r])
        for kc in range(16):
            b = kc % 2
            if out_dram is None:
                cx.op("dve", lambda e, kc=kc: e.scalar_tensor_tensor(
                    out=hT[:, kc, :], in0=hT[:, kc, :], scalar=gt[:, kc:kc + 1], in1=rstd[:], op0=ALU.mult, op1=ALU.mult),
                    reads=[rstd_r, gres], writes=[hres[kc]])
            else:
                cx.dma("sp", xin[b][:], xsrc[kc * 128:(kc + 1) * 128, :], reads=[self.xres_r[kc]], writes=[xin_r[b]])
                cx.op("dve", lambda e, b=b, kc=kc: e.scalar_tensor_tensor(
                    out=xin[b][:], in0=xin[b][:], scalar=gt[:, kc:kc + 1], in1=rstd[:], op0=ALU.mult, op1=ALU.mult),
                    reads=[xin_r[b], rstd_r, gres], writes=[xin_r[b]])
                cx.dma("sp", out_dram[kc * 128:(kc + 1) * 128, :], xin[b][:], reads=[xin_r[b]])
        return m

    def phase_proj(self, l, xsrc):
        cx = self.cx
        cx.barrier()
        cx.release(self.persist_mark)
        hT = cx.sb([128, 16, S], BF16, "hT")
        hres = [Res("hT%d" % i) for i in range(16)]
        bg = cx.sb([128, 48], F32, "bg")
        bgres = Res("bg")
        cx.dma("sp", bg[:], self.b_gate[l], writes=[bgres])
        m = self.rmsnorm_to_sbuf(xsrc, self.g_mix[l], hT, hres)
        if "hdbg" in self.taps:
            for kc in range(16):
                cx.dma("sp", self.hdbg[kc * 128:(kc + 1) * 128, :], hT[:, kc, :], reads=[hres[kc]])
        cx.release(m)
        ost = [cx.sb([128, S], BF16, "ost") for _ in range(3)]
        ost_r = [Res("ost") for _ in range(3)]
        vst = [cx.sb([128, 512], BF16, "vst") for _ in range(3)]
        vst_r = [Res("vst") for _ in range(3)]
        opos = 0
        vpos = 0
        wv = self.w_in[l].rearrange("(kc p) n -> p kc n", p=128)
        for cg in range(30):
            c0 = cg * 512
            s = self.next_slot()
            slot = self.wslot[s][:, 0:8192].rearrange("p (kc n) -> p kc n", kc=16)
            cx.dma("pool", slot, wv[:, :, c0:c0 + 512], writes=[self.wres[s]])
            kind = "g" if cg >= 18 else ("q", "q", "k", "k", "v", "v")[cg % 6]
            br = (cg // 6) if cg < 18 else None
            if kind == "v":
                vcol0 = (cg % 6 - 4) * 512
                for tb in range(16):
                    pst, pr = self.bank()

                    def mm(e, tb=tb, pst=pst, slot=slot):
                        ins = None
                        for kc in range(16):
                            ins = e.matmul(pst[:], hT[:, kc, tb * 128:(tb + 1) * 128], slot[:, kc, :],
                                           start=(kc == 0), stop=(kc == 15))
                        return ins
                    cx.op("pe", mm, reads=[self.wres[s]] + hres, writes=[pr])
                    vb = vpos % 3
                    vpos += 1
                    cx.op("dve", lambda e, vb=vb, pst=pst: e.tensor_copy(out=vst[vb][:], in_=pst[:]),
                          reads=[pr], writes=[vst_r[vb]])
                    cx.dma("sp", self.vtok[br, tb * 128:(tb + 1) * 128, vcol0:vcol0 + 512], vst[vb][:],
                           reads=[vst_r[vb]])
                continue
            for sub in range(4):
                ob = opos % 3
                opos += 1
                col = c0 + sub * 128
                for tg in range(4):
                    pst, pr = self.bank()

                    def mm(e, sub=sub, tg=tg, pst=pst, slot=slot):
                        ins = None
                        for kc in range(16):
                            ins = e.matmul(pst[:], slot[:, kc, sub * 128:(sub + 1) * 128],
                                           hT[:, kc, tg * 512:(tg + 1) * 512], start=(kc == 0), stop=(kc == 15))
                        return ins
                    cx.op("pe", mm, reads=[self.wres[s]] + hres, writes=[pr])
                    dst = ost[ob][:, tg * 512:(tg + 1) * 512]
                    if kind == "g":
                        j = (col - 9216) // 128
                        cx.op("act", lambda e, dst=dst, pst=pst, j=j: e.activation(
                            out=dst, in_=pst[:], func=AF.Sigmoid, bias=bg[:, j:j + 1], scale=1.0),
                            reads=[pr, bgres], writes=[ost_r[ob]])
                    elif kind == "q":
                        sc = 0.125 if br == 0 else 128.0 ** -0.5
                        cx.op("dve", lambda e, dst=dst, pst=pst, sc=sc: e.tensor_scalar(
                            out=dst, in0=pst[:], scalar1=sc, scalar2=None, op0=ALU.mult),
                            reads=[pr], writes=[ost_r[ob]])
                    else:
                        cx.op("dve", lambda e, dst=dst, pst=pst: e.tensor_copy(out=dst, in_=pst[:]),
                              reads=[pr], writes=[ost_r[ob]])
                if kind == "g":
                    cx.dma("sp", self.gateT[col - 9216:col - 9216 + 128, :], ost[ob][:], reads=[ost_r[ob]])
                else:
                    cx.dma("sp", self.projT[col:col + 128, :], ost[ob][:], reads=[ost_r[ob]])


    def load_v(self, br):
        cx = self.cx
        vt = cx.sb([128, 16, 1024], BF16, "vt")
        vres = Res("vt")
        src = self.vtok[br].rearrange("(blk p) c -> p blk c", p=128)
        cx.dma("pool", vt[:], src, writes=[vres], phase_local=True)
        return vt, vres

    def run_pipeline(self, tasks, stages, skews):
        n = len(tasks)
        self.deferred = []
        maxs = max(skews)
        for step in range(n + maxs + 8):
            for st, sk in zip(stages, skews):
                i = step - sk
                if 0 <= i < n:
                    st(tasks[i], step)
            keep = []
            for due, fn in self.deferred:
                if due <= step:
                    fn()
                else:
                    keep.append((due, fn))
            self.deferred = keep
        assert not self.deferred

    def defer(self, due, fn):
        self.deferred.append((due, fn))

    def phase_attn_a(self, l):
        cx = self.cx
        cx.barrier()
        cx.release(self.persist_mark)
        lam_init = 0.8 - 0.6 * math.exp(-0.3 * l)
        cf, cb = self.cf, self.cb
        ones = self.ones()
        lv = cx.sb([128, 4, 64], F32, "lv")
        prod = cx.sb([128, 2, 64], F32, "prod")
        sm = cx.sb([128, 8], F32, "sm")
        sw = cx.sb([128, 1], F32, "sw")
        lres = Res("lam")
        cx.dma("sp", lv[:], self.lamv[l].partition_broadcast(128), writes=[lres])
        cx.dma("sp", sw[:], self.subln[l], writes=[lres])
        cx.op("dve", lambda e: e.tensor_tensor(out=prod[:, 0, :], in0=lv[:, 0, :], in1=lv[:, 1, :], op=ALU.mult),
              reads=[lres], writes=[lres])
        cx.op("dve", lambda e: e.tensor_tensor(out=prod[:, 1, :], in0=lv[:, 2, :], in1=lv[:, 3, :], op=ALU.mult),
              reads=[lres], writes=[lres])
        cx.op("dve", lambda e: e.reduce_sum(out=sm[:, 0:2], in_=prod[:], axis=AX.X), reads=[lres], writes=[lres])
        cx.op("act", lambda e: e.activation(out=sm[:, 2:4], in_=sm[:, 0:2], func=AF.Exp), reads=[lres], writes=[lres])
        cx.op("dve", lambda e: e.tensor_tensor(out=sm[:, 4:5], in0=sm[:, 3:4], in1=sm[:, 2:3], op=ALU.subtract),
              reads=[lres], writes=[lres])
        cx.op("dve", lambda e: e.tensor_scalar(out=sm[:, 5:6], in0=sm[:, 4:5], scalar1=-lam_init, scalar2=None, op0=ALU.add),
              reads=[lres], writes=[lres])
        cx.op("dve", lambda e: e.tensor_scalar(out=sm[:, 6:7], in0=sw[:, 0:1], scalar1=1.0 - lam_init, scalar2=None, op0=ALU.mult),
              reads=[lres], writes=[lres])
        nlam = sm[:, 5:6]
        wsc = sm[:, 6:7]

        vt, vres = self.load_v(0)
        qk = [cx.sb([65, 4, S], BF16, "qkA") for _ in range(2)]
        qkres = [Res("qkA") for _ in range(2)]
        oh = [cx.sb([128, S], BF16, "ohA") for _ in range(2)]
        ohres = [Res("ohA") for _ in range(2)]
        NP = 4
        pt = [cx.sb([128, 512], BF16, "ptA") for _ in range(NP)]
        ptres = [Res("ptA") for _ in range(NP)]
        rcp = cx.sb([128, 512], F32, "rcpA")
        rcpres = Res("rcpA")
        tn = [cx.sb([128, 512], F32, "tnA") for _ in range(2)]
        tnres = [Res("tnA") for _ in range(2)]
        o32 = cx.sb([128, 512], F32, "o32A")
        o32res = Res("o32A")
        rs = cx.sb([128, 512], F32, "rsA")
        rsres = Res("rsA")
        sqb = cx.sb([128, 512], BF16, "sqA")
        sqres = Res("sqA")

        def load_head(h):
            hb = h % 2
            r0 = h * 128
            cx.dma("sp", qk[hb][0:64, 0:2, :], self.projT[r0:r0 + 128, :].rearrange("(m p) s -> p m s", p=64),
                   writes=[qkres[hb]])
            cx.dma("sp", qk[hb][0:64, 2:4, :], self.projT[1024 + r0:1024 + r0 + 128, :].rearrange("(m p) s -> p m s", p=64),
                   writes=[qkres[hb]])
            for m in range(2):
                cx.dma("sp", qk[hb][64:65, m, :], self.c_rows[h:h + 1, :], writes=[qkres[hb]])
                cx.dma("sp", qk[hb][64:65, 2 + m, :], self.c_rows[8:9, :], writes=[qkres[hb]])

        tasks = []
        u = 0
        for h in range(8):
            for c in range(4):
                nkb = 4 * c + 4
                for m in range(2):
                    for kb in range(nkb):
                        tasks.append(dict(h=h, c=c, m=m, kb=kb, nkb=nkb, u=u, first_of_head=(c == 0 and m == 0 and kb == 0),
                                          last_of_head=(c == 3 and m == 1 and kb == nkb - 1)))
                    u += 1
        self.sctr = 0
        load_head(0)

        def s1(t, step):
            h, c, m, kb = t["h"], t["c"], t["m"], t["kb"]
            hb = h % 2
            if t["first_of_head"] and h + 1 < 8:
                load_head(h + 1)
            d = kb - 4 * c
            c0 = 128 * d if d > 0 else 0
            bS = self.sctr % 4
            self.sctr += 1
            S_ = self.ps[bS]
            p_ = t["p"] = bS
            t["c0"] = c0
            cx.op("pe", lambda e: e.matmul(S_[:, c0:512], qk[hb][0:65, 2 + m, kb * 128:(kb + 1) * 128],
                                           qk[hb][0:65, m, c * 512 + c0:(c + 1) * 512], start=True, stop=True),
                  reads=[qkres[hb]], writes=[self.pres[bS]])
            if d >= 0:
                cx.op("dve", lambda e: e.tensor_tensor(
                    out=S_[:, 128 * d:128 * d + 128], in0=S_[:, 128 * d:128 * d + 128],
                    in1=cf[:, CF_TRINEG:CF_TRINEG + 128], op=ALU.add),
                    reads=[self.pres[bS], self.cres], writes=[self.pres[bS]])
            bcol = CF_KPA + h * 16 + kb
            cx.op("act", lambda e: e.activation(out=pt[p_][:, c0:512], in_=S_[:, c0:512], func=AF.Exp,
                                                bias=cf[:, bcol:bcol + 1], scale=1.0),
                  reads=[self.pres[bS], self.cres], writes=[ptres[p_]])

        def s2(t, step):
            h, c, m, kb, nkb, u_ = t["h"], t["c"], t["m"], t["kb"], t["nkb"], t["u"]
            hb = h % 2
            p_, c0 = t["p"], t["c0"]
            bO = 4 + 2 * (u_ % 2)
            bSm = bO + 1

            def mmO(e):
                e.matmul(self.ps[bO][:, c0:512], vt[:, kb, h * 128:(h + 1) * 128], pt[p_][:, c0:512],
                         start=(kb == 0), stop=(kb == nkb - 1))
                return e.matmul(self.ps[bSm][:, c0:512], ones, pt[p_][:, c0:512], start=(kb == 0), stop=(kb == nkb - 1))
            cx.op("pe", mmO, reads=[ptres[p_], vres, self.cres], writes=[self.pres[bO], self.pres[bSm]])
            if kb != nkb - 1:
                return
            def fin0():
                cx.op("act", lambda e: e.activation(out=rcp[:], in_=self.ps[bSm][:], func=AF.Ln), reads=[self.pres[bSm]], writes=[rcpres])
                cx.op("act", lambda e: e.activation(out=rcp[:], in_=rcp[:], func=AF.Exp, scale=-1.0), reads=[rcpres], writes=[rcpres])
                cx.op("dve", lambda e: e.tensor_tensor(out=tn[m][:], in0=self.ps[bO][:], in1=rcp[:], op=ALU.mult),
                      reads=[self.pres[bO], rcpres], writes=[tnres[m]])
            self.defer(step + 1, fin0)
            if m == 0:
                return

            def fin1():
                cx.op("dve", lambda e: e.scalar_tensor_tensor(out=o32[:], in0=tn[1][:], scalar=nlam, in1=tn[0][:],
                                                              op0=ALU.mult, op1=ALU.add),
                      reads=[tnres[0], tnres[1], lres], writes=[o32res])

            def fin2():
                cx.op("act", lambda e: e.activation(out=sqb[:], in_=o32[:], func=AF.Square), reads=[o32res], writes=[sqres])
                bq = self.sctr % 4
                self.sctr += 1
                t["bq"] = bq
                cx.op("pe", lambda e: e.matmul(self.ps[bq][:], ones, sqb[:], start=True, stop=True),
                      reads=[sqres, self.cres], writes=[self.pres[bq]])

            def fin3():
                bq = t["bq"]
                cx.op("act", lambda e: e.activation(out=rs[:], in_=self.ps[bq][:], func=AF.Ln,
                                                    bias=cf[:, CF_EPS:CF_EPS + 1], scale=1.0 / 128),
                      reads=[self.pres[bq], self.cres], writes=[rsres])
                cx.op("act", lambda e: e.activation(out=rs[:], in_=rs[:], func=AF.Exp, scale=-0.5), reads=[rsres], writes=[rsres])
                cx.op("dve", lambda e: e.scalar_tensor_tensor(
                    out=oh[hb][:, c * 512:(c + 1) * 512], in0=o32[:], scalar=wsc, in1=rs[:], op0=ALU.mult, op1=ALU.mult),
                    reads=[o32res, rsres, lres], writes=[ohres[hb]])
                if t["last_of_head"]:
                    cx.dma("sp", self.oT[0, h * 128:(h + 1) * 128, :], oh[hb][:], reads=[ohres[hb]])
            self.defer(step + 2, fin1)
            self.defer(step + 3, fin2)
            self.defer(step + 5, fin3)

        self.run_pipeline(tasks, [s1, s2], [0, 2])

    def phase_attn_b(self, l):
        cx = self.cx
        cx.barrier()
        cx.release(self.persist_mark)
        cf, cb = self.cf, self.cb
        ones = self.ones()
        tri = cb[:, CB_TRI:CB_TRI + 128]
        vt, vres = self.load_v(1)
        qk = [cx.sb([128, 3, S], BF16, "qkB") for _ in range(2)]
        qkres = [Res("qkB") for _ in range(2)]
        oh = [cx.sb([128, S], BF16, "ohB") for _ in range(2)]
        ohres = [Res("ohB") for _ in range(2)]
        NB = 3
        ef = [cx.sb([128, 512], F32, "eB") for _ in range(NB)]
        efres = [Res("eB") for _ in range(NB)]
        lb = [cx.sb([128, 512], BF16, "lB") for _ in range(NB)]
        lbres = [Res("lB") for _ in range(NB)]
        tmp = [cx.sb([128, 512], F32, "tB") for _ in range(2)]
        tmpres = [Res("tB") for _ in range(2)]
        pt = [cx.sb([128, 512], BF16, "ptB") for _ in range(NB)]
        ptres = [Res("ptB") for _ in range(NB)]
        cs = cx.sb([128, 512], F32, "csB")
        csres = Res("csB")

        def load_head(h):
            hb = h % 2
            r0 = 3072 + h * 128
            cx.dma("sp", qk[hb][:, 0, :], self.projT[r0:r0 + 128, :], writes=[qkres[hb]])
            cx.dma("sp", qk[hb][:, 1, :], self.projT[r0 + 1024:r0 + 1024 + 128, :], writes=[qkres[hb]])
            cx.op("dve", lambda e: e.tensor_scalar(out=qk[hb][:, 2, :], in0=qk[hb][:, 1, :], scalar1=-1.0,
                                                   scalar2=None, op0=ALU.mult),
                  reads=[qkres[hb]], writes=[qkres[hb]])

        tasks = []
        u = 0
        i = 0
        for h in range(8):
            for c in range(4):
                nkb = 4 * c + 4
                for kb in range(nkb - 1, -1, -1):
                    tasks.append(dict(h=h, c=c, kb=kb, nkb=nkb, u=u, i=i, first_of_head=(c == 0 and kb == nkb - 1),
                                      last_of_head=(c == 3 and kb == 0)))
                    i += 1
                u += 1
        load_head(0)

        def s1(t, step):
            h, c, kb, i = t["h"], t["c"], t["kb"], t["i"]
            hb = h % 2
            if t["first_of_head"] and h + 1 < 8:
                load_head(h + 1)
            d = kb - 4 * c
            c0 = t["c0"] = 128 * d if d > 0 else 0
            j = i % NB
            bZ = i % 2
            Z = self.ps[bZ]
            qs = slice(c * 512 + c0, (c + 1) * 512)
            ks = slice(kb * 128, (kb + 1) * 128)
            cx.op("pe", lambda e: e.matmul(Z[:, c0:512], qk[hb][:, 1, ks], qk[hb][:, 0, qs], start=True, stop=True),
                  reads=[qkres[hb]], writes=[self.pres[bZ]])
            cx.op("act", lambda e: e.activation(out=ef[j][:, c0:512], in_=Z[:, c0:512], func=AF.Exp),
                  reads=[self.pres[bZ]], writes=[efres[j]])
            cx.op("act", lambda e: e.activation(out=lb[j][:, c0:512], in_=ef[j][:, c0:512], func=AF.Ln,
                                                bias=cf[:, CF_ONE:CF_ONE + 1], scale=1.0),
                  reads=[efres[j], self.cres], writes=[lbres[j]])
            if d >= 0:
                cx.op("dve", lambda e: e.tensor_tensor(
                    out=lb[j][:, 128 * d:128 * d + 128], in0=lb[j][:, 128 * d:128 * d + 128],
                    in1=cb[:, CB_M01S:CB_M01S + 128], op=ALU.mult),
                    reads=[lbres[j], self.cres], writes=[lbres[j]])

        def s2(t, step):
            h, c, kb, nkb, i = t["h"], t["c"], t["kb"], t["nkb"], t["i"]
            hb = h % 2
            c0 = t["c0"]
            d = kb - 4 * c
            j = i % NB
            j2 = i % 2
            bX, bC = 2 + (i % 2), 4 + (i % 2)
            X, CBk = self.ps[bX], self.ps[bC]
            qs = slice(c * 512 + c0, (c + 1) * 512)
            ks = slice(kb * 128, (kb + 1) * 128)
            if kb == nkb - 1:
                cx.op("dve", lambda e: e.memset(cs[:], 0.0), writes=[csres])

            def mmX(e):
                e.matmul(X[:, c0:512], tri, lb[j][:, c0:512], start=True, stop=False)
                e.matmul(X[:, c0:512], qk[hb][:, 2, ks], qk[hb][:, 0, qs], start=False, stop=True)
                return e.matmul(CBk[:, c0:512], ones, lb[j][:, c0:512], start=True, stop=True)
            cx.op("pe", mmX, reads=[lbres[j], qkres[hb], self.cres], writes=[self.pres[bX], self.pres[bC]])
            cx.op("dve", lambda e: e.tensor_tensor(out=tmp[j2][:, c0:512], in0=X[:, c0:512], in1=cs[:, c0:512], op=ALU.add),
                  reads=[self.pres[bX], csres], writes=[tmpres[j2]])
            if d >= 0:
                cx.op("dve", lambda e: e.tensor_tensor(
                    out=tmp[j2][:, 128 * d:128 * d + 128], in0=tmp[j2][:, 128 * d:128 * d + 128],
                    in1=cf[:, CF_TRIPOS:CF_TRIPOS + 128], op=ALU.add),
                    reads=[tmpres[j2], self.cres], writes=[tmpres[j2]])
            cx.op("act", lambda e: e.activation(out=pt[j][:, c0:512], in_=tmp[j2][:, c0:512], func=AF.Exp, scale=-1.0),
                  reads=[tmpres[j2]], writes=[ptres[j]])
            if kb > 0:
                cx.op("dve", lambda e: e.tensor_tensor(out=cs[:, c0:512], in0=CBk[:, c0:512], in1=cs[:, c0:512], op=ALU.add),
                      reads=[self.pres[bC], csres], writes=[csres])

        def s3(t, step):
            h, c, kb, nkb, i, u_ = t["h"], t["c"], t["kb"], t["nkb"], t["i"], t["u"]
            hb = h % 2
            c0 = t["c0"]
            j = i % NB
            bO = 6 + (u_ % 2)
            cx.op("pe", lambda e: e.matmul(self.ps[bO][:, c0:512], vt[:, kb, h * 128:(h + 1) * 128], pt[j][:, c0:512],
                                           start=(kb == nkb - 1), stop=(kb == 0)),
                  reads=[ptres[j], vres], writes=[self.pres[bO]])
            if kb == 0:
                def fin():
                    cx.op("act", lambda e: e.activation(out=oh[hb][:, c * 512:(c + 1) * 512], in_=self.ps[bO][:], func=AF.Copy),
                          reads=[self.pres[bO]], writes=[ohres[hb]])
                    if t["last_of_head"]:
                        cx.dma("sp", self.oT[1, h * 128:(h + 1) * 128, :], oh[hb][:], reads=[ohres[hb]])
                self.defer(step + 1, fin)

        self.run_pipeline(tasks, [s1, s2, s3], [0, 2, 4])

    def phase_attn_c(self, l):
        cx = self.cx
        cx.barrier()
        cx.release(self.persist_mark)
        cf, cb = self.cf, self.cb
        ones = self.ones()
        ident = cb[:, CB_IDENT:CB_IDENT + 128]
        vt, vres = self.load_v(2)
        ltab = cx.sb([9, S], BF16, "ltab")
        ltres = Res("ltab")
        cx.dma("sp", ltab[:], self.c_rows[17:26, :], writes=[ltres])
        qk = [cx.sb([128, 2, S], BF16, "qkC") for _ in range(2)]
        qkres = [Res("qkC") for _ in range(2)]
        rt = [cx.sb([9, S], BF16, "rtC") for _ in range(2)]
        rtres = [Res("rtC") for _ in range(2)]
        oh = [cx.sb([128, S], BF16, "ohC") for _ in range(2)]
        ohres = [Res("ohC") for _ in range(2)]
        NP = 3
        pt = [cx.sb([128, 512], BF16, "ptC") for _ in range(NP)]
        ptres = [Res("ptC") for _ in range(NP)]
        km = cx.sb([128, 8], F32, "km")
        kh = cx.sb([128, 8], BF16, "kh")
        kl = cx.sb([128, 8], BF16, "kl")
        gm = cx.sb([128, 128], F32, "gm")
        mx = cx.sb([128, 128], F32, "mx")
        sel = cx.sb([128, 128], F32, "sel")
        mb = cx.sb([128, 128], BF16, "mb")
        gres = Res("gsel")
        rr = cx.sb([128, 512], F32, "rrC")
        rrres = Res("rrC")

        def load_head(h):
            hb = h % 2
            r0 = 6144 + h * 128
            cx.dma("sp", qk[hb][:, 0, :], self.projT[r0:r0 + 128, :], writes=[qkres[hb]])
            cx.dma("sp", qk[hb][:, 1, :], self.projT[r0 + 1024:r0 + 1024 + 128, :], writes=[qkres[hb]])
            cx.dma("sp", rt[hb][8:9, :], self.c_rows[9 + h:10 + h, :], writes=[rtres[hb]])

        def gate_head(h, gstep=None):
            hb = h % 2
            cx.op("dve", lambda e: e.tensor_reduce(out=km[:], in_=qk[hb][:, 1, :].rearrange("p (n k) -> p n k", k=256),
                                                   axis=AX.X, op=ALU.add),
                  reads=[qkres[hb]], writes=[gres])
            cx.op("dve", lambda e: e.tensor_copy(out=kh[:], in_=km[:]), reads=[gres], writes=[gres])
            cx.op("dve", lambda e: e.tensor_tensor(out=kl[:], in0=km[:], in1=kh[:], op=ALU.subtract), reads=[gres], writes=[gres])
            bG = 6

            def mmG(e):
                ins = None
                for qb in range(16):
                    e.matmul(self.ps[bG][:, qb * 8:(qb + 1) * 8], qk[hb][:, 0, qb * 128:(qb + 1) * 128], kh[:], start=True, stop=False)
                    ins = e.matmul(self.ps[bG][:, qb * 8:(qb + 1) * 8], qk[hb][:, 0, qb * 128:(qb + 1) * 128], kl[:], start=False, stop=True)
                return ins
            cx.op("pe", mmG, reads=[qkres[hb], gres], writes=[self.pres[bG]])
            cx.op("dve", lambda e: e.tensor_tensor(out=gm[:], in0=self.ps[bG][:, 0:128], in1=cf[:, CF_PAD:CF_PAD + 128], op=ALU.add),
                  reads=[self.pres[bG], self.cres], writes=[gres])
            for qb in range(16):
                cx.op("dve", lambda e, qb=qb: e.max(out=mx[:, qb * 8:(qb + 1) * 8], in_=gm[:, qb * 8:(qb + 1) * 8]),
                      reads=[gres], writes=[gres])
            for qb in range(16):
                cx.op("dve", lambda e, qb=qb: e.tensor_scalar(
                    out=sel[:, qb * 8:(qb + 1) * 8], in0=gm[:, qb * 8:(qb + 1) * 8], scalar1=mx[:, qb * 8 + 2:qb * 8 + 3],
                    scalar2=-NEG, op0=ALU.is_ge, op1=ALU.mult), reads=[gres], writes=[gres])
            cx.op("dve", lambda e: e.scalar_tensor_tensor(out=mb[:], in0=sel[:], scalar=NEG, in1=cf[:, CF_PAST:CF_PAST + 128],
                                                          op0=ALU.add, op1=ALU.mult),
                  reads=[gres, self.cres], writes=[gres])
            def transposes():
              for half in range(2):
                bT = 6 + half
                tb = self.ps[bT][:, :].bitcast(BF16)

                def mmT(e, half=half, tb=tb):
                    ins = None
                    for q8 in range(8):
                        qb = half * 8 + q8
                        ins = e.transpose(tb[0:8, q8 * 128:(q8 + 1) * 128], mb[:, qb * 8:(qb + 1) * 8], ident)
                    return ins
                cx.op("pe", mmT, reads=[gres, self.cres], writes=[self.pres[bT]])
                cx.op("act", lambda e, half=half, tb=tb: e.activation(
                    out=rt[hb][0:8, half * 1024:(half + 1) * 1024], in_=tb[0:8, :], func=AF.Copy),
                    reads=[self.pres[bT]], writes=[rtres[hb]])
            if gstep is None:
                transposes()
            else:
                self.defer(gstep + 14, transposes)

        tasks = []
        u = 0
        i = 0
        for h in range(8):
            for c in range(4):
                nkb = 4 * c + 4
                for kb in range(nkb):
                    tasks.append(dict(h=h, c=c, kb=kb, nkb=nkb, u=u, i=i, first_of_head=(c == 0 and kb == 0),
                                      mid_of_head=(c == 2 and kb == 0), last_of_head=(c == 3 and kb == nkb - 1)))
                    i += 1
                u += 1
        load_head(0)
        gate_head(0)

        def s1(t, step):
            h, c, kb, i = t["h"], t["c"], t["kb"], t["i"]
            hb = h % 2
            if t["first_of_head"] and h + 1 < 8:
                load_head(h + 1)
            if t["mid_of_head"] and h + 1 < 8:
                gate_head(h + 1, step)
            d = kb - 4 * c
            c0 = t["c0"] = 128 * d if d > 0 else 0
            bS = i % 2
            S_ = self.ps[bS]
            p_ = i % NP
            qs = slice(c * 512 + c0, (c + 1) * 512)
            ks = slice(kb * 128, (kb + 1) * 128)

            def mmS(e):
                e.matmul(S_[:, c0:512], qk[hb][:, 1, ks], qk[hb][:, 0, qs], start=True, stop=False)
                return e.matmul(S_[:, c0:512], ltab[0:9, ks], rt[hb][0:9, qs], start=False, stop=True)
            cx.op("pe", mmS, reads=[qkres[hb], rtres[hb], ltres], writes=[self.pres[bS]])
            if d >= 0:
                cx.op("dve", lambda e: e.tensor_tensor(
                    out=S_[:, 128 * d:128 * d + 128], in0=S_[:, 128 * d:128 * d + 128],
                    in1=cf[:, CF_TRINEG:CF_TRINEG + 128], op=ALU.add),
                    reads=[self.pres[bS], self.cres], writes=[self.pres[bS]])
            bcol = CF_KPC + h * 16 + kb
            cx.op("act", lambda e: e.activation(out=pt[p_][:, c0:512], in_=S_[:, c0:512], func=AF.Exp,
                                                bias=cf[:, bcol:bcol + 1], scale=1.0),
                  reads=[self.pres[bS], self.cres], writes=[ptres[p_]])

        def s2(t, step):
            h, c, kb, nkb, i, u_ = t["h"], t["c"], t["kb"], t["nkb"], t["i"], t["u"]
            hb = h % 2
            c0 = t["c0"]
            p_ = i % NP
            bO = 2 + 2 * (u_ % 2)
            bSm = bO + 1

            def mmO(e):
                e.matmul(self.ps[bO][:, c0:512], vt[:, kb, h * 128:(h + 1) * 128], pt[p_][:, c0:512],
                         start=(kb == 0), stop=(kb == nkb - 1))
                return e.matmul(self.ps[bSm][:, c0:512], ones, pt[p_][:, c0:512], start=(kb == 0), stop=(kb == nkb - 1))
            cx.op("pe", mmO, reads=[ptres[p_], vres, self.cres], writes=[self.pres[bO], self.pres[bSm]])
            if kb == nkb - 1:
                def fin():
                    cx.op("act", lambda e: e.activation(out=rr[:], in_=self.ps[bSm][:], func=AF.Ln), reads=[self.pres[bSm]], writes=[rrres])
                    cx.op("act", lambda e: e.activation(out=rr[:], in_=rr[:], func=AF.Exp, scale=-1.0), reads=[rrres], writes=[rrres])
                    cx.op("dve", lambda e: e.tensor_tensor(out=oh[hb][:, c * 512:(c + 1) * 512], in0=self.ps[bO][:], in1=rr[:],
                                                           op=ALU.mult),
                          reads=[self.pres[bO], rrres], writes=[ohres[hb]])
                    if t["last_of_head"]:
                        cx.dma("sp", self.oT[2, h * 128:(h + 1) * 128, :], oh[hb][:], reads=[ohres[hb]])
                self.defer(step + 1, fin)

        self.run_pipeline(tasks, [s1, s2], [0, 1])

    def phase_merge(self, l):
        cx = self.cx
        cx.barrier()
        cx.release(self.persist_mark)
        oall = cx.sb([128, 24, S], BF16, "oall")
        ores = [Res("oall%d" % i) for i in range(3)]
        for i in range(3):
            cx.dma("pool", oall[:, i * 8:(i + 1) * 8, :], self.oT[i].rearrange("(hc p) s -> p hc s", p=128), writes=[ores[i]], phase_local=True)
        gsb = [cx.sb([128, 3, S], BF16, "gsb") for _ in range(2)]
        gsres = [Res("gsb") for _ in range(2)]
        mst = [cx.sb([128, S], BF16, "mst") for _ in range(2)]
        mstres = [Res("mst") for _ in range(2)]
        ta = cx.sb([128, 512], F32, "ta")
        tbb = cx.sb([128, 512], F32, "tb")
        tares, tbres = Res("ta"), Res("tb")
        wv = self.w_branch[l].rearrange("i (hc p) n -> p i hc n", p=128)
        bi = 0
        for cg in range(4):
            c0 = cg * 512
            s = self.next_slot()
            slot = self.wslot[s][:, 0:12288].rearrange("p (i hc n) -> p i hc n", i=3, hc=8)
            for i in range(3):
                cx.dma("pool", slot[:, i, :, :], wv[:, i, :, c0:c0 + 512], writes=[self.wres[s]])
            for sub in range(4):
                b = bi % 2
                bi += 1
                dblk = cg * 4 + sub
                for i in range(3):
                    cx.dma("sp", gsb[b][:, i, :], self.gateT[i * 2048 + dblk * 128:i * 2048 + (dblk + 1) * 128, :],
                           writes=[gsres[b]])
                for tg in range(4):
                    zb = []
                    for i in range(3):
                        pst, pr = self.bank()
                        zb.append((pst, pr))

                        def mm(e, i=i, pst=pst, slot=slot, sub=sub, tg=tg):
                            ins = None
                            for hc in range(8):
                                ins = e.matmul(pst[:], slot[:, i, hc, sub * 128:(sub + 1) * 128],
                                               oall[:, i * 8 + hc, tg * 512:(tg + 1) * 512], start=(hc == 0), stop=(hc == 7))
                            return ins
                        cx.op("pe", mm, reads=[self.wres[s], ores[i]], writes=[pr])
                    ts = slice(tg * 512, (tg + 1) * 512)
                    cx.op("dve", lambda e, b=b, ts=ts, z=zb[0][0]: e.tensor_tensor(out=ta[:], in0=z[:], in1=gsb[b][:, 0, ts], op=ALU.mult),
                          reads=[zb[0][1], gsres[b]], writes=[tares])
                    cx.op("dve", lambda e, b=b, ts=ts, z=zb[1][0]: e.tensor_tensor(out=tbb[:], in0=z[:], in1=gsb[b][:, 1, ts], op=ALU.mult),
                          reads=[zb[1][1], gsres[b]], writes=[tbres])
                    cx.op("dve", lambda e: e.tensor_tensor(out=ta[:], in0=ta[:], in1=tbb[:], op=ALU.add),
                          reads=[tares, tbres], writes=[tares])
                    cx.op("dve", lambda e, b=b, ts=ts, z=zb[2][0]: e.tensor_tensor(out=tbb[:], in0=z[:], in1=gsb[b][:, 2, ts], op=ALU.mult),
                          reads=[zb[2][1], gsres[b]], writes=[tbres])
                    cx.op("dve", lambda e, b=b, ts=ts: e.tensor_tensor(out=mst[b][:, ts], in0=ta[:], in1=tbb[:], op=ALU.add),
                          reads=[tares, tbres], writes=[mstres[b]])
                cx.dma("sp", self.mergedT[dblk * 128:(dblk + 1) * 128, :], mst[b][:], reads=[mstres[b]])

    def residual_gemm(self, xsrc, w_ap, a_dram, nkc, npass=1):
        cx = self.cx
        cx.barrier()
        cx.release(self.persist_mark)
        nb = 1 if npass == 1 else 2
        aT = [cx.sb([128, nkc, S], BF16, "aT") for _ in range(nb)]
        ares = [Res("aT") for _ in range(nb)]
        xr = [cx.sb([128, S], F32, "xr") for _ in range(3)]
        xrres = [Res("xr") for _ in range(3)]
        rows = nkc * 128

        def load_a(p):
            cx.dma("pool", aT[p % nb][:], a_dram[p * rows:(p + 1) * rows, :].rearrange("(kc p) s -> p kc s", p=128),
                   writes=[ares[p % nb]], phase_local=(p < 2))
        load_a(0)
        xi = 0
        for p in range(npass):
            if p + 1 < npass:
                load_a(p + 1)
            wv = w_ap[p * rows:(p + 1) * rows, :].rearrange("(kc p) n -> p kc n", p=128)
            src_x = xsrc if p == 0 else self.xres
            at = aT[p % nb]
            for cg in range(4):
                c0 = cg * 512
                s = self.next_slot()
                slot = self.wslot[s][:, 0:nkc * 512].rearrange("p (kc n) -> p kc n", kc=nkc)
                cx.dma("pool", slot, wv[:, :, c0:c0 + 512], writes=[self.wres[s]])
                for sub in range(4):
                    blk = cg * 4 + sub
                    b = xi % 3
                    xi += 1
                    cx.dma("sp", xr[b][:], src_x[blk * 128:(blk + 1) * 128, :], reads=[self.xres_r[blk]], writes=[xrres[b]])
                    for tg in range(4):
                        pst, pr = self.bank()

                        def mm(e, pst=pst, slot=slot, sub=sub, tg=tg, at=at):
                            ins = None
                            for kc in range(nkc):
                                ins = e.matmul(pst[:], slot[:, kc, sub * 128:(sub + 1) * 128], at[:, kc, tg * 512:(tg + 1) * 512],
                                               start=(kc == 0), stop=(kc == nkc - 1))
                            return ins
                        cx.op("pe", mm, reads=[self.wres[s], ares[p % nb]], writes=[pr])
                        ts = slice(tg * 512, (tg + 1) * 512)
                        cx.op("dve", lambda e, b=b, ts=ts, pst=pst: e.tensor_tensor(out=xr[b][:, ts], in0=pst[:], in1=xr[b][:, ts], op=ALU.add),
                              reads=[pr, xrres[b]], writes=[xrres[b]])
                    cx.dma("sp", self.xres[blk * 128:(blk + 1) * 128, :], xr[b][:], reads=[xrres[b]], writes=[self.xres_r[blk]])

    def phase_ffn_up(self, l):
        cx = self.cx
        cx.barrier()
        cx.release(self.persist_mark)
        hT = cx.sb([128, 16, S], BF16, "hT2")
        hres = [Res("hT2_%d" % i) for i in range(16)]
        m = self.rmsnorm_to_sbuf(self.xres, self.g_ffn[l], hT, hres)
        cx.release(m)
        ast = [cx.sb([128, S], BF16, "ast") for _ in range(2)]
        astres = [Res("ast") for _ in range(2)]
        sg = [cx.sb([128, 512], F32, "sg") for _ in range(2)]
        sgres = [Res("sg") for _ in range(2)]
        wg = self.w_gate[l].rearrange("(kc p) n -> p kc n", p=128)
        wu = self.w_up[l].rearrange("(kc p) n -> p kc n", p=128)
        ai = 0
        si = 0
        for fg in range(22):
            f0 = fg * 256
            s = self.next_slot()
            slot = self.wslot[s][:, 0:8192].rearrange("p (g kc n) -> p g kc n", g=2, kc=16)
            cx.dma("pool", slot[:, 0, :, :], wg[:, :, f0:f0 + 256], writes=[self.wres[s]])
            cx.dma("pool", slot[:, 1, :, :], wu[:, :, f0:f0 + 256], writes=[self.wres[s]])
            for sub in range(2):
                b = ai % 2
                ai += 1
                fblk = fg * 2 + sub
                for tg in range(4):
                    pg, prg = self.bank()
                    pu, pru = self.bank()

                    def mm(e, pg=pg, pu=pu, slot=slot, sub=sub, tg=tg):
                        ins = None
                        for kc in range(16):
                            e.matmul(pg[:], slot[:, 0, kc, sub * 128:(sub + 1) * 128], hT[:, kc, tg * 512:(tg + 1) * 512],
                                     start=(kc == 0), stop=(kc == 15))
                        for kc in range(16):
                            ins = e.matmul(pu[:], slot[:, 1, kc, sub * 128:(sub + 1) * 128], hT[:, kc, tg * 512:(tg + 1) * 512],
                                           start=(kc == 0), stop=(kc == 15))
                        return ins
                    cx.op("pe", mm, reads=[self.wres[s]] + hres, writes=[prg, pru])
                    j = si % 2
                    si += 1
                    cx.op("act", lambda e, j=j, pg=pg: e.activation(out=sg[j][:], in_=pg[:], func=AF.Silu),
                          reads=[prg], writes=[sgres[j]])
                    ts = slice(tg * 512, (tg + 1) * 512)
                    cx.op("dve", lambda e, j=j, b=b, ts=ts, pu=pu: e.tensor_tensor(out=ast[b][:, ts], in0=pu[:], in1=sg[j][:], op=ALU.mult),
                          reads=[pru, sgres[j]], writes=[astres[b]])
                cx.dma("sp", self.actT[fblk * 128:(fblk + 1) * 128, :], ast[b][:], reads=[astres[b]])

    def phase_final(self, xsrc):
        cx = self.cx
        cx.barrier()
        cx.release(self.persist_mark)
        self.rmsnorm_to_sbuf(xsrc, self.g_fin, None, None, out_dram=self.outT)


CB_ONES = 0
CB_TRI = 128
CB_IDENT = 256
CB_M01S = 384
CB_COLS = 512
CF_EPS = 0
CF_ONE = 1
CF_KPA = 8
CF_KPC = 136
CF_TRINEG = 264
CF_TRIPOS = 392
CF_PAD = 520
CF_PAST = 648
CF_COLS = 776
NROWS = 26


def make_consts():
    p = np.arange(128)
    cb = np.zeros((128, CB_COLS), dtype=np.float32)
    cb[:, CB_ONES:CB_ONES + 128] = 1.0
    cb[:, CB_TRI:CB_TRI + 128] = (p[:, None] >= p[None, :])
    cb[:, CB_IDENT:CB_IDENT + 128] = np.eye(128)
    cb[:, CB_M01S:CB_M01S + 128] = (p[:, None] < p[None, :])
    cf = np.zeros((128, CF_COLS), dtype=np.float32)
    cf[:, CF_EPS] = EPS
    cf[:, CF_ONE] = 1.0
    for h in range(8):
        for kb in range(16):
            cf[:, CF_KPA + h * 16 + kb] = ALIBI_DIFF[h] * (kb * 128 + p)
            cf[:, CF_KPC + h * 16 + kb] = ALIBI_MOBA[h] * (kb * 128 + p)
    cf[:, CF_TRINEG:CF_TRINEG + 128] = np.where(p[:, None] <= p[None, :], 0.0, NEG)
    cf[:, CF_TRIPOS:CF_TRIPOS + 128] = np.where(p[:, None] < p[None, :], 0.0, -NEG)
    for qb in range(16):
        own = qb // 2
        for n in range(8):
            cf[:, CF_PAD + qb * 8 + n] = 0.0 if n < own else -3.0e38
            cf[:, CF_PAST + qb * 8 + n] = 1.0 if n < own else 0.0
    t = np.arange(S, dtype=np.float64)
    rows = np.zeros((NROWS, S), dtype=np.float32)
    for h in range(8):
        rows[h] = -ALIBI_DIFF[h] * t
        rows[9 + h] = -ALIBI_MOBA[h] * t
    rows[8] = 1.0
    for n in range(8):
        rows[17 + n] = (np.arange(S) // 256 == n)
    rows[25] = 1.0
    return cb.astype(ml_dtypes.bfloat16), cf, rows.astype(ml_dtypes.bfloat16)


def make_inputs(inp, b):
    cb, cf, rows = make_consts()
    f = lambda a: np.ascontiguousarray(a, dtype=np.float32)
    d = {
        "xT": f(inp["x"][b].T),
        "w_in": f(inp["w_in"]), "w_branch": f(inp["w_branch"]), "w_out": f(inp["w_out"]),
        "w_ffn_gate": f(inp["w_ffn_gate"]), "w_ffn_up": f(inp["w_ffn_up"]), "w_ffn_down": f(inp["w_ffn_down"]),
        "g_mix": f(inp["norm_mix_g"].reshape(DEPTH, 16, 128).transpose(0, 2, 1)),
        "g_ffn": f(inp["norm_ffn_g"].reshape(DEPTH, 16, 128).transpose(0, 2, 1)),
        "g_fin": f(inp["final_norm_g"].reshape(16, 128).T),
        "b_gate": f(inp["b_gate"].reshape(DEPTH, 48, 128).transpose(0, 2, 1)),
        "lamv": f(np.stack([inp["lam_q1"], inp["lam_k1"], inp["lam_q2"], inp["lam_k2"]], axis=1)),
        "subln": f(inp["subln_w"].reshape(DEPTH, 128, 1)),
        "c_bf": cb, "c_f32": cf, "c_rows": rows,
    }
    return d


def kernel(**inputs):
    prog = Prog()
    n = 8
    shared = None
    in_maps = []
    for b in range(n):
        d = make_inputs(inputs, b) if shared is None else dict(shared, xT=np.ascontiguousarray(inputs["x"][b].T, dtype=np.float32))
        if shared is None:
            shared = d
        in_maps.append(d)
    res = run_bass_kernel_spmd(prog.nc, in_maps, core_ids=list(range(n)))
    out = np.stack([np.ascontiguousarray(r["outT"].T) for r in res.results], axis=0)
    return out.astype(np.float32)
```

```python
import math
import numpy as np
import ml_dtypes
import concourse.bass as bass
import concourse.mybir as mybir
from concourse.bass_utils import run_bass_kernel_spmd

F32 = mybir.dt.float32
BF16 = mybir.dt.bfloat16
AF = mybir.ActivationFunctionType
ALU = mybir.AluOpType
AX = mybir.AxisListType

D = 2048
S = 2048
DEPTH = 2
NCOLS = 15360
DFF = 5632
EPS = 1e-6
NEG = -30000.0
N_ALIBI = 16
ALIBI_ALL = 2.0 ** (-8.0 * (np.arange(N_ALIBI) + 1) / N_ALIBI)
ALIBI_DIFF = ALIBI_ALL[0::2].astype(np.float32)
ALIBI_MOBA = ALIBI_ALL[1::2].astype(np.float32)

SB_BASE = 16512
SB_TOP = 229344


class Res:
    __slots__ = ("name", "w", "r")

    def __init__(self, name=""):
        self.name = name
        self.w = None
        self.r = []


class Stream:
    def __init__(self, name):
        self.name = name
        self.items = []
        self.count = 0
        self.sem = None
        self.waited = {}
        self.ring = []
        self.ring_pos = 0


class Ctx:
    COMPUTE = ("pe", "act", "dve", "pool")

    def __init__(self, nc):
        self.nc = nc
        self.st = {n: Stream(n) for n in ("pe", "act", "dve", "pool", "sp")}
        for n in self.COMPUTE:
            self.st[n].sem = nc.alloc_semaphore("c_" + n)
        for n, k in (("sp", 4), ("pool", 4), ("act", 2)):
            for i in range(k):
                self.st[n].ring.append([nc.alloc_semaphore("d_%s%d" % (n, i)), 0, None])
        self.sb_off = SB_BASE
        self.last_barrier = []
        self.nalloc = 0
        self.bank = 0

    def sb(self, shape, dtype, name="t"):
        nbytes = int(np.prod(shape[1:])) * (4 if dtype == F32 else 2)
        nbytes = (nbytes + 31) // 32 * 32
        assert self.sb_off + nbytes <= SB_TOP, ("sbuf overflow", name, self.sb_off, nbytes)
        self.nalloc += 1
        t = self.nc.alloc_sbuf_tensor_at("%s_%d" % (name, self.nalloc), list(shape), dtype, offset=self.sb_off)
        self.sb_off += nbytes
        return t

    def mark(self):
        return self.sb_off

    def release(self, m):
        self.sb_off = m

    def _need(self, st, tok, waits):
        if tok is None:
            return
        kind, key, val, sem = tok
        if kind == "c" and key == "pe" and st.name == "pe":
            return
        k = (kind, key)
        if st.waited.get(k, 0) >= val:
            return
        st.waited[k] = val
        waits.append((sem, val))

    def _collect(self, st, reads, writes):
        waits = []
        for r in reads:
            self._need(st, r.w, waits)
        for w in writes:
            self._need(st, w.w, waits)
            for t in w.r:
                self._need(st, t, waits)
        return waits

    def _update(self, tok, reads, writes):
        for r in reads:
            r.r.append(tok)
        for w in writes:
            w.w = tok
            w.r = []

    def op(self, eng, fn, reads=(), writes=()):
        st = self.st[eng]
        waits = self._collect(st, reads, writes)
        st.count += 1
        tok = ("c", eng, st.count, st.sem)
        st.items.append((waits, fn, (st.sem, 1)))
        self._update(tok, reads, writes)
        return tok

    def dma(self, q, out, in_, reads=(), writes=(), phase_local=False):
        st = self.st[q]
        waits = self._collect(st, reads, writes)
        if phase_local:
            for t in self.last_barrier:
                if not (t[0] == "c" and t[1] == q):
                    self._need(st, t, waits)
        slot = st.ring[st.ring_pos % len(st.ring)]
        st.ring_pos += 1
        self._need(st, slot[2], waits)
        slot[1] += 16
        tok = ("d", slot[0].num, slot[1], slot[0])
        slot[2] = tok
        st.items.append((waits, (lambda e, o=out, i=in_: e.dma_start(out=o, in_=i)), (slot[0], 16)))
        self._update(tok, reads, writes)
        return tok

    def barrier(self, engines=("pe", "act", "dve", "sp")):
        toks = []
        for n in self.COMPUTE:
            s = self.st[n]
            if s.count:
                toks.append(("c", n, s.count, s.sem))
        for n in ("sp", "pool", "act"):
            for slot in self.st[n].ring:
                if slot[2] is not None:
                    toks.append(slot[2])
        self.last_barrier = toks
        for n in engines:
            st = self.st[n]
            waits = []
            for t in toks:
                if t[0] == "c" and t[1] == n:
                    continue
                self._need(st, t, waits)
            if waits:
                st.items.append((waits, None, None))

    def next_bank(self):
        b = self.bank
        self.bank = (self.bank + 1) % 8
        return b

    def emit(self):
        nc = self.nc
        st = self.st

        def run(s, e):
            for waits, fn, inc in s.items:
                for sem, val in waits:
                    e.wait_ge(sem, val)
                if fn is not None:
                    ins = fn(e)
                    ins.then_inc(inc[0], inc[1])

        with nc.Block() as block:
            @block.tensor
            def _(e):
                run(st["pe"], e)

            @block.scalar
            def _(e):
                run(st["act"], e)

            @block.vector
            def _(e):
                run(st["dve"], e)

            @block.gpsimd
            def _(e):
                run(st["pool"], e)

            @block.sync
            def _(e):
                run(st["sp"], e)


class Prog:
    def __init__(self, n_layers=DEPTH, stop_after=None, taps=(), skip=()):
        self.skip = set(skip)
        self.n_layers = n_layers
        self.stop_after = stop_after
        self.taps = set(taps)
        nc = self.nc = bass.Bass("TRN2", target_bir_lowering=False)
        self.cx = Ctx(nc)
        ein = lambda name, shape, dt=F32: nc.dram_tensor(name, list(shape), dt, kind="ExternalInput").ap()
        self.xT = ein("xT", [D, S])
        self.w_in = ein("w_in", [DEPTH, D, NCOLS])
        self.w_branch = ein("w_branch", [DEPTH, 3, 1024, D])
        self.w_out = ein("w_out", [DEPTH, D, D])
        self.w_gate = ein("w_ffn_gate", [DEPTH, D, DFF])
        self.w_up = ein("w_ffn_up", [DEPTH, D, DFF])
        self.w_down = ein("w_ffn_down", [DEPTH, DFF, D])
        self.g_mix = ein("g_mix", [DEPTH, 128, 16])
        self.g_ffn = ein("g_ffn", [DEPTH, 128, 16])
        self.g_fin = ein("g_fin", [128, 16])
        self.b_gate = ein("b_gate", [DEPTH, 128, 48])
        self.lamv = ein("lamv", [DEPTH, 4, 64])
        self.subln = ein("subln", [DEPTH, 128, 1])
        self.c_bf = ein("c_bf", [128, CB_COLS], BF16)
        self.c_f32 = ein("c_f32", [128, CF_COLS])
        self.c_rows = ein("c_rows", [NROWS, S], BF16)
        self.outT = nc.dram_tensor("outT", [D, S], F32, kind="ExternalOutput").ap()

        def scratch(name, shape, dt):
            kind = "ExternalOutput" if name in self.taps else "Internal"
            return nc.dram_tensor(name, list(shape), dt, kind=kind).ap()
        self.xres = scratch("xres", [D, S], F32)
        self.projT = scratch("projT", [9216, S], BF16)
        self.vtok = scratch("vtok", [3, S, 1024], BF16)
        self.gateT = scratch("gateT", [6144, S], BF16)
        self.oT = scratch("oT", [3, 1024, S], BF16)
        self.mergedT = scratch("mergedT", [D, S], BF16)
        self.actT = scratch("actT", [DFF, S], BF16)
        self.hdbg = scratch("hdbg", [D, S], BF16)

        cx = self.cx
        self.cb = cx.sb([128, CB_COLS], BF16, "cb")
        self.cf = cx.sb([128, CF_COLS], F32, "cf")
        self.cres = Res("consts")
        cx.dma("sp", self.cb[:], self.c_bf[:, :], writes=[self.cres])
        cx.dma("sp", self.cf[:], self.c_f32[:, :], writes=[self.cres])
        self.NSLOT = 2
        self.wslot = [cx.sb([128, 12288], BF16, "wslot") for _ in range(self.NSLOT)]
        self.wres = [Res("wslot%d" % i) for i in range(self.NSLOT)]
        self.wpos = 0
        self.ps = [nc.alloc_psum_tensor("ps%d" % i, [128, 512], F32) for i in range(8)]
        self.pres = [Res("ps%d" % i) for i in range(8)]
        self.xres_r = [Res("xres%d" % i) for i in range(16)]
        self.persist_mark = cx.mark()

        self.build()
        cx.barrier(engines=("pe", "act", "dve", "pool", "sp"))
        cx.emit()

    def ones(self):
        return self.cb[:, CB_ONES:CB_ONES + 128]

    def next_slot(self):
        s = self.wpos % self.NSLOT
        self.wpos += 1
        return s

    def bank(self):
        b = self.cx.next_bank()
        return self.ps[b], self.pres[b]

    def build(self):
        xsrc = self.xT
        for l in range(self.n_layers):
            steps = [
                ("proj", lambda: self.phase_proj(l, xsrc)),
                ("attn_a", lambda: self.phase_attn_a(l)),
                ("attn_b", lambda: self.phase_attn_b(l)),
                ("attn_c", lambda: self.phase_attn_c(l)),
                ("merge", lambda: self.phase_merge(l)),
                ("wout", lambda: self.residual_gemm(xsrc, self.w_out[l], self.mergedT, 16)),
                ("ffn_up", lambda: self.phase_ffn_up(l)),
            ]
            for name, fn in steps:
                if name in self.skip:
                    continue
                fn()
                if self.stop_after == (name, l):
                    return
            self.residual_gemm(self.xres, self.w_down[l], self.actT, 11, npass=4)
            if self.stop_after == ("ffn_down", l):
                return
            xsrc = self.xres
        self.phase_final(xsrc)

    def rmsnorm_to_sbuf(self, xsrc, g_ap, hT, hres, out_dram=None):
        cx = self.cx
        m = cx.mark()
        gt = cx.sb([128, 16], F32, "g")
        gres = Res("g")
        cx.dma("sp", gt[:], g_ap, writes=[gres])
        xin = [cx.sb([128, S], F32, "xin") for _ in range(2)]
        xin_r = [Res("xin") for _ in range(2)]
        sq = [cx.sb([128, S], BF16, "sq") for _ in range(2)]
        sq_r = [Res("sq") for _ in range(2)]
        rstd = cx.sb([128, S], F32, "rstd")
        rstd_r = Res("rstd")
        banks = [self.bank() for _ in range(4)]
        ones = self.ones()
        for kc in range(16):
            b = kc % 2
            cx.dma("sp", xin[b][:], xsrc[kc * 128:(kc + 1) * 128, :], reads=[self.xres_r[kc]], writes=[xin_r[b]])
            cx.op("act", lambda e, b=b: e.activation(out=sq[b][:], in_=xin[b][:], func=AF.Square),
                  reads=[xin_r[b]], writes=[sq_r[b]])

            def mm(e, b=b, kc=kc):
                ins = None
                for tg in range(4):
                    ins = e.matmul(banks[tg][0][:], ones, sq[b][:, tg * 512:(tg + 1) * 512],
                                   start=(kc == 0), stop=(kc == 15))
                return ins
            cx.op("pe", mm, reads=[sq_r[b], self.cres], writes=[bk[1] for bk in banks])
            if out_dram is None:
                cx.op("dve", lambda e, b=b, kc=kc: e.tensor_copy(out=hT[:, kc, :], in_=xin[b][:]),
                      reads=[xin_r[b]], writes=[hres[kc]])
        for tg in range(4):
            cx.op("act", lambda e, tg=tg: e.activation(out=rstd[:, tg * 512:(tg + 1) * 512], in_=banks[tg][0][:],
                                                        func=AF.Ln, bias=self.cf[:, CF_EPS:CF_EPS + 1], scale=1.0 / D),
                  reads=[banks[tg][1], self.cres], writes=[rstd_r])
        cx.op("act", lambda e: e.activation(out=rstd[:], in_=rstd[:], func=AF.Exp, scale=-0.5), reads=[rstd_r], writes=[rstd_r])
        for kc in range(16):
            b = kc % 2
            if out_dram is None:
                cx.op("dve", lambda e, kc=kc: e.scalar_tensor_tensor(
                    out=hT[:, kc, :], in0=hT[:, kc, :], scalar=gt[:, kc:kc + 1], in1=rstd[:], op0=ALU.mult, op1=ALU.mult),
                    reads=[rstd_r, gres], writes=[hres[kc]])
            else:
                cx.dma("sp", xin[b][:], xsrc[kc * 128:(kc + 1) * 128, :], reads=[self.xres_r[kc]], writes=[xin_r[b]])
                cx.op("dve", lambda e, b=b, kc=kc: e.scalar_tensor_tensor(
                    out=xin[b][:], in0=xin[b][:], scalar=gt[:, kc:kc + 1], in1=rstd[:], op0=ALU.mult, op1=ALU.mult),
                    reads=[xin_r[b], rstd_r, gres], writes=[xin_r[b]])
                cx.dma("sp", out_dram[kc * 128:(kc + 1) * 128, :], xin[b][:], reads=[xin_r[b]])
        return m

    def phase_proj(self, l, xsrc):
        cx = self.cx
        cx.barrier()
        cx.release(self.persist_mark)
        hT = cx.sb([128, 16, S], BF16, "hT")
        hres = [Res("hT%d" % i) for i in range(16)]
        bg = cx.sb([128, 48], F32, "bg")
        bgres = Res("bg")
        cx.dma("sp", bg[:], self.b_gate[l], writes=[bgres])
        m = self.rmsnorm_to_sbuf(xsrc, self.g_mix[l], hT, hres)
        if "hdbg" in self.taps:
            for kc in range(16):
                cx.dma("sp", self.hdbg[kc * 128:(kc + 1) * 128, :], hT[:, kc, :], reads=[hres[kc]])
        cx.release(m)
        ost = [cx.sb([128, S], BF16, "ost") for _ in range(3)]
        ost_r = [Res("ost") for _ in range(3)]
        vst = [cx.sb([128, 512], BF16, "vst") for _ in range(3)]
        vst_r = [Res("vst") for _ in range(3)]
        opos = 0
        vpos = 0
        wv = self.w_in[l].rearrange("(kc p) n -> p kc n", p=128)
        for cg in range(30):
            c0 = cg * 512
            s = self.next_slot()
            slot = self.wslot[s][:, 0:8192].rearrange("p (kc n) -> p kc n", kc=16)
            cx.dma("pool", slot, wv[:, :, c0:c0 + 512], writes=[self.wres[s]])
            kind = "g" if cg >= 18 else ("q", "q", "k", "k", "v", "v")[cg % 6]
            br = (cg // 6) if cg < 18 else None
            if kind == "v":
                vcol0 = (cg % 6 - 4) * 512
                for tb in range(16):
                    pst, pr = self.bank()

                    def mm(e, tb=tb, pst=pst, slot=slot):
                        ins = None
                        for kc in range(16):
                            ins = e.matmul(pst[:], hT[:, kc, tb * 128:(tb + 1) * 128], slot[:, kc, :],
                                           start=(kc == 0), stop=(kc == 15))
                        return ins
                    cx.op("pe", mm, reads=[self.wres[s]] + hres, writes=[pr])
                    vb = vpos % 3
                    vpos += 1
                    cx.op("dve", lambda e, vb=vb, pst=pst: e.tensor_copy(out=vst[vb][:], in_=pst[:]),
                          reads=[pr], writes=[vst_r[vb]])
                    cx.dma("sp", self.vtok[br, tb * 128:(tb + 1) * 128, vcol0:vcol0 + 512], vst[vb][:],
                           reads=[vst_r[vb]])
                continue
            for sub in range(4):
                ob = opos % 3
                opos += 1
                col = c0 + sub * 128
                for tg in range(4):
                    pst, pr = self.bank()

                    def mm(e, sub=sub, tg=tg, pst=pst, slot=slot):
                        ins = None
                        for kc in range(16):
                            ins = e.matmul(pst[:], slot[:, kc, sub * 128:(sub + 1) * 128],
                                           hT[:, kc, tg * 512:(tg + 1) * 512], start=(kc == 0), stop=(kc == 15))
                        return ins
                    cx.op("pe", mm, reads=[self.wres[s]] + hres, writes=[pr])
                    dst = ost[ob][:, tg * 512:(tg + 1) * 512]
                    if kind == "g":
                        j = (col - 9216) // 128
                        cx.op("act", lambda e, dst=dst, pst=pst, j=j: e.activation(
                            out=dst, in_=pst[:], func=AF.Sigmoid, bias=bg[:, j:j + 1], scale=1.0),
                            reads=[pr, bgres], writes=[ost_r[ob]])
                    elif kind == "q":
                        sc = 0.125 if br == 0 else 128.0 ** -0.5
                        cx.op("dve", lambda e, dst=dst, pst=pst, sc=sc: e.tensor_scalar(
                            out=dst, in0=pst[:], scalar1=sc, scalar2=None, op0=ALU.mult),
                            reads=[pr], writes=[ost_r[ob]])
                    else:
                        cx.op("dve", lambda e, dst=dst, pst=pst: e.tensor_copy(out=dst, in_=pst[:]),
                              reads=[pr], writes=[ost_r[ob]])
                if kind == "g":
                    cx.dma("sp", self.gateT[col - 9216:col - 9216 + 128, :], ost[ob][:], reads=[ost_r[ob]])
                else:
                    cx.dma("sp", self.projT[col:col + 128, :], ost[ob][:], reads=[ost_r[ob]])


    def load_v(self, br):
        cx = self.cx
        vt = cx.sb([128, 16, 1024], BF16, "vt")
        vres = Res("vt")
        src = self.vtok[br].rearrange("(blk p) c -> p blk c", p=128)
        cx.dma("pool", vt[:], src, writes=[vres], phase_local=True)
        return vt, vres

    def run_pipeline(self, tasks, stages, skews):
        n = len(tasks)
        self.deferred = []
        maxs = max(skews)
        for step in range(n + maxs + 8):
            for st, sk in zip(stages, skews):
                i = step - sk
                if 0 <= i < n:
                    st(tasks[i], step)
            keep = []
            for due, fn in self.deferred:
                if due <= step:
                    fn()
                else:
                    keep.append((due, fn))
            self.deferred = keep
        assert not self.deferred

    def defer(self, due, fn):
        self.deferred.append((due, fn))

    def phase_attn_a(self, l):
        cx = self.cx
        cx.barrier()
        cx.release(self.persist_mark)
        lam_init = 0.8 - 0.6 * math.exp(-0.3 * l)
        cf, cb = self.cf, self.cb
        ones = self.ones()
        lv = cx.sb([128, 4, 64], F32, "lv")
        prod = cx.sb([128, 2, 64], F32, "prod")
        sm = cx.sb([128, 8], F32, "sm")
        sw = cx.sb([128, 1], F32, "sw")
        lres = Res("lam")
        cx.dma("sp", lv[:], self.lamv[l].partition_broadcast(128), writes=[lres])
        cx.dma("sp", sw[:], self.subln[l], writes=[lres])
        cx.op("dve", lambda e: e.tensor_tensor(out=prod[:, 0, :], in0=lv[:, 0, :], in1=lv[:, 1, :], op=ALU.mult),
              reads=[lres], writes=[lres])
        cx.op("dve", lambda e: e.tensor_tensor(out=prod[:, 1, :], in0=lv[:, 2, :], in1=lv[:, 3, :], op=ALU.mult),
              reads=[lres], writes=[lres])
        cx.op("dve", lambda e: e.reduce_sum(out=sm[:, 0:2], in_=prod[:], axis=AX.X), reads=[lres], writes=[lres])
        cx.op("act", lambda e: e.activation(out=sm[:, 2:4], in_=sm[:, 0:2], func=AF.Exp), reads=[lres], writes=[lres])
        cx.op("dve", lambda e: e.tensor_tensor(out=sm[:, 4:5], in0=sm[:, 3:4], in1=sm[:, 2:3], op=ALU.subtract),
              reads=[lres], writes=[lres])
        cx.op("dve", lambda e: e.tensor_scalar(out=sm[:, 5:6], in0=sm[:, 4:5], scalar1=-lam_init, scalar2=None, op0=ALU.add),
              reads=[lres], writes=[lres])
        cx.op("dve", lambda e: e.tensor_scalar(out=sm[:, 6:7], in0=sw[:, 0:1], scalar1=1.0 - lam_init, scalar2=None, op0=ALU.mult),
              reads=[lres], writes=[lres])
        nlam = sm[:, 5:6]
        wsc = sm[:, 6:7]

        vt, vres = self.load_v(0)
        qk = [cx.sb([65, 4, S], BF16, "qkA") for _ in range(2)]
        qkres = [Res("qkA") for _ in range(2)]
        oh = [cx.sb([128, S], BF16, "ohA") for _ in range(2)]
        ohres = [Res("ohA") for _ in range(2)]
        NP = 4
        pt = [cx.sb([128, 512], BF16, "ptA") for _ in range(NP)]
        ptres = [Res("ptA") for _ in range(NP)]
        rcp = cx.sb([128, 512], F32, "rcpA")
        rcpres = Res("rcpA")
        tn = [cx.sb([128, 512], F32, "tnA") for _ in range(2)]
        tnres = [Res("tnA") for _ in range(2)]
        o32 = cx.sb([128, 512], F32, "o32A")
        o32res = Res("o32A")
        rs = cx.sb([128, 512], F32, "rsA")
        rsres = Res("rsA")
        sqb = cx.sb([128, 512], BF16, "sqA")
        sqres = Res("sqA")

        def load_head(h):
            hb = h % 2
            r0 = h * 128
            cx.dma("sp", qk[hb][0:64, 0:2, :], self.projT[r0:r0 + 128, :].rearrange("(m p) s -> p m s", p=64),
                   writes=[qkres[hb]])
            cx.dma("sp", qk[hb][0:64, 2:4, :], self.projT[1024 + r0:1024 + r0 + 128, :].rearrange("(m p) s -> p m s", p=64),
                   writes=[qkres[hb]])
            for m in range(2):
                cx.dma("sp", qk[hb][64:65, m, :], self.c_rows[h:h + 1, :], writes=[qkres[hb]])
                cx.dma("sp", qk[hb][64:65, 2 + m, :], self.c_rows[8:9, :], writes=[qkres[hb]])

        tasks = []
        u = 0
        for h in range(8):
            for c in range(4):
                nkb = 4 * c + 4
                for m in range(2):
                    for kb in range(nkb):
                        tasks.append(dict(h=h, c=c, m=m, kb=kb, nkb=nkb, u=u, first_of_head=(c == 0 and m == 0 and kb == 0),
                                          last_of_head=(c == 3 and m == 1 and kb == nkb - 1)))
                    u += 1
        self.sctr = 0
        load_head(0)

        def s1(t, step):
            h, c, m, kb = t["h"], t["c"], t["m"], t["kb"]
            hb = h % 2
            if t["first_of_head"] and h + 1 < 8:
                load_head(h + 1)
            d = kb - 4 * c
            c0 = 128 * d if d > 0 else 0
            bS = self.sctr % 4
            self.sctr += 1
            S_ = self.ps[bS]
            p_ = t["p"] = bS
            t["c0"] = c0
            cx.op("pe", lambda e: e.matmul(S_[:, c0:512], qk[hb][0:65, 2 + m, kb * 128:(kb + 1) * 128],
                                           qk[hb][0:65, m, c * 512 + c0:(c + 1) * 512], start=True, stop=True),
                  reads=[qkres[hb]], writes=[self.pres[bS]])
            if d >= 0:
                cx.op("dve", lambda e: e.tensor_tensor(
                    out=S_[:, 128 * d:128 * d + 128], in0=S_[:, 128 * d:128 * d + 128],
                    in1=cf[:, CF_TRINEG:CF_TRINEG + 128], op=ALU.add),
                    reads=[self.pres[bS], self.cres], writes=[self.pres[bS]])
            bcol = CF_KPA + h * 16 + kb
            cx.op("act", lambda e: e.activation(out=pt[p_][:, c0:512], in_=S_[:, c0:512], func=AF.Exp,
                                                bias=cf[:, bcol:bcol + 1], scale=1.0),
                  reads=[self.pres[bS], self.cres], writes=[ptres[p_]])

        def s2(t, step):
            h, c, m, kb, nkb, u_ = t["h"], t["c"], t["m"], t["kb"], t["nkb"], t["u"]
            hb = h % 2
            p_, c0 = t["p"], t["c0"]
            bO = 4 + 2 * (u_ % 2)
            bSm = bO + 1

            def mmO(e):
                e.matmul(self.ps[bO][:, c0:512], vt[:, kb, h * 128:(h + 1) * 128], pt[p_][:, c0:512],
                         start=(kb == 0), stop=(kb == nkb - 1))
                return e.matmul(self.ps[bSm][:, c0:512], ones, pt[p_][:, c0:512], start=(kb == 0), stop=(kb == nkb - 1))
            cx.op("pe", mmO, reads=[ptres[p_], vres, self.cres], writes=[self.pres[bO], self.pres[bSm]])
            if kb != nkb - 1:
                return
            def fin0():
                cx.op("act", lambda e: e.activation(out=rcp[:], in_=self.ps[bSm][:], func=AF.Ln), reads=[self.pres[bSm]], writes=[rcpres])
                cx.op("act", lambda e: e.activation(out=rcp[:], in_=rcp[:], func=AF.Exp, scale=-1.0), reads=[rcpres], writes=[rcpres])
                cx.op("dve", lambda e: e.tensor_tensor(out=tn[m][:], in0=self.ps[bO][:], in1=rcp[:], op=ALU.mult),
                      reads=[self.pres[bO], rcpres], writes=[tnres[m]])
            self.defer(step + 1, fin0)
            if m == 0:
                return

            def fin1():
                cx.op("dve", lambda e: e.scalar_tensor_tensor(out=o32[:], in0=tn[1][:], scalar=nlam, in1=tn[0][:],
                                                              op0=ALU.mult, op1=ALU.add),
                      reads=[tnres[0], tnres[1], lres], writes=[o32res])

            def fin2():
                cx.op("act", lambda e: e.activation(out=sqb[:], in_=o32[:], func=AF.Square), reads=[o32res], writes=[sqres])
                bq = self.sctr % 4
                self.sctr += 1
                t["bq"] = bq
                cx.op("pe", lambda e: e.matmul(self.ps[bq][:], ones, sqb[:], start=True, stop=True),
                      reads=[sqres, self.cres], writes=[self.pres[bq]])

            def fin3():
                bq = t["bq"]
                cx.op("act", lambda e: e.activation(out=rs[:], in_=self.ps[bq][:], func=AF.Ln,
                                                    bias=cf[:, CF_EPS:CF_EPS + 1], scale=1.0 / 128),
                      reads=[self.pres[bq], self.cres], writes=[rsres])
                cx.op("act", lambda e: e.activation(out=rs[:], in_=rs[:], func=AF.Exp, scale=-0.5), reads=[rsres], writes=[rsres])
                cx.op("dve", lambda e: e.scalar_tensor_tensor(
                    out=oh[hb][:, c * 512:(c + 1) * 512], in0=o32[:], scalar=wsc, in1=rs[:], op0=ALU.mult, op1=ALU.mult),
                    reads=[o32res, rsres, lres], writes=[ohres[hb]])
                if t["last_of_head"]:
                    cx.dma("sp", self.oT[0, h * 128:(h + 1) * 128, :], oh[hb][:], reads=[ohres[hb]])
            self.defer(step + 2, fin1)
            self.defer(step + 3, fin2)
            self.defer(step + 5, fin3)

        self.run_pipeline(tasks, [s1, s2], [0, 2])

    def phase_attn_b(self, l):
        cx = self.cx
        cx.barrier()
        cx.release(self.persist_mark)
        cf, cb = self.cf, self.cb
        ones = self.ones()
        tri = cb[:, CB_TRI:CB_TRI + 128]
        vt, vres = self.load_v(1)
        qk = [cx.sb([128, 3, S], BF16, "qkB") for _ in range(2)]
        qkres = [Res("qkB") for _ in range(2)]
        oh = [cx.sb([128, S], BF16, "ohB") for _ in range(2)]
        ohres = [Res("ohB") for _ in range(2)]
        NB = 3
        ef = [cx.sb([128, 512], F32, "eB") for _ in range(NB)]
        efres = [Res("eB") for _ in range(NB)]
        lb = [cx.sb([128, 512], BF16, "lB") for _ in range(NB)]
        lbres = [Res("lB") for _ in range(NB)]
        tmp = [cx.sb([128, 512], F32, "tB") for _ in range(2)]
        tmpres = [Res("tB") for _ in range(2)]
        pt = [cx.sb([128, 512], BF16, "ptB") for _ in range(NB)]
        ptres = [Res("ptB") for _ in range(NB)]
        cs = cx.sb([128, 512], F32, "csB")
        csres = Res("csB")

        def load_head(h):
            hb = h % 2
            r0 = 3072 + h * 128
            cx.dma("sp", qk[hb][:, 0, :], self.projT[r0:r0 + 128, :], writes=[qkres[hb]])
            cx.dma("sp", qk[hb][:, 1, :], self.projT[r0 + 1024:r0 + 1024 + 128, :], writes=[qkres[hb]])
            cx.op("dve", lambda e: e.tensor_scalar(out=qk[hb][:, 2, :], in0=qk[hb][:, 1, :], scalar1=-1.0,
                                                   scalar2=None, op0=ALU.mult),
                  reads=[qkres[hb]], writes=[qkres[hb]])

        tasks = []
        u = 0
        i = 0
        for h in range(8):
            for c in range(4):
                nkb = 4 * c + 4
                for kb in range(nkb - 1, -1, -1):
                    tasks.append(dict(h=h, c=c, kb=kb, nkb=nkb, u=u, i=i, first_of_head=(c == 0 and kb == nkb - 1),
                                      last_of_head=(c == 3 and kb == 0)))
                    i += 1
                u += 1
        load_head(0)

        def s1(t, step):
            h, c, kb, i = t["h"], t["c"], t["kb"], t["i"]
            hb = h % 2
            if t["first_of_head"] and h + 1 < 8:
                load_head(h + 1)
            d = kb - 4 * c
            c0 = t["c0"] = 128 * d if d > 0 else 0
            j = i % NB
            bZ = i % 2
            Z = self.ps[bZ]
            qs = slice(c * 512 + c0, (c + 1) * 512)
            ks = slice(kb * 128, (kb + 1) * 128)
            cx.op("pe", lambda e: e.matmul(Z[:, c0:512], qk[hb][:, 1, ks], qk[hb][:, 0, qs], start=True, stop=True),
                  reads=[qkres[hb]], writes=[self.pres[bZ]])
            cx.op("act", lambda e: e.activation(out=ef[j][:, c0:512], in_=Z[:, c0:512], func=AF.Exp),
                  reads=[self.pres[bZ]], writes=[efres[j]])
            cx.op("act", lambda e: e.activation(out=lb[j][:, c0:512], in_=ef[j][:, c0:512], func=AF.Ln,
                                                bias=cf[:, CF_ONE:CF_ONE + 1], scale=1.0),
                  reads=[efres[j], self.cres], writes=[lbres[j]])
            if d >= 0:
                cx.op("dve", lambda e: e.tensor_tensor(
                    out=lb[j][:, 128 * d:128 * d + 128], in0=lb[j][:, 128 * d:128 * d + 128],
                    in1=cb[:, CB_M01S:CB_M01S + 128], op=ALU.mult),
                    reads=[lbres[j], self.cres], writes=[lbres[j]])

        def s2(t, step):
            h, c, kb, nkb, i = t["h"], t["c"], t["kb"], t["nkb"], t["i"]
            hb = h % 2
            c0 = t["c0"]
            d = kb - 4 * c
            j = i % NB
            j2 = i % 2
            bX, bC = 2 + (i % 2), 4 + (i % 2)
            X, CBk = self.ps[bX], self.ps[bC]
            qs = slice(c * 512 + c0, (c + 1) * 512)
            ks = slice(kb * 128, (kb + 1) * 128)
            if kb == nkb - 1:
                cx.op("dve", lambda e: e.memset(cs[:], 0.0), writes=[csres])

            def mmX(e):
                e.matmul(X[:, c0:512], tri, lb[j][:, c0:512], start=True, stop=False)
                e.matmul(X[:, c0:512], qk[hb][:, 2, ks], qk[hb][:, 0, qs], start=False, stop=True)
                return e.matmul(CBk[:, c0:512], ones, lb[j][:, c0:512], start=True, stop=True)
            cx.op("pe", mmX, reads=[lbres[j], qkres[hb], self.cres], writes=[self.pres[bX], self.pres[bC]])
            cx.op("dve", lambda e: e.tensor_tensor(out=tmp[j2][:, c0:512], in0=X[:, c0:512], in1=cs[:, c0:512], op=ALU.add),
                  reads=[self.pres[bX], csres], writes=[tmpres[j2]])
            if d >= 0:
                cx.op("dve", lambda e: e.tensor_tensor(
                    out=tmp[j2][:, 128 * d:128 * d + 128], in0=tmp[j2][:, 128 * d:128 * d + 128],
                    in1=cf[:, CF_TRIPOS:CF_TRIPOS + 128], op=ALU.add),
                    reads=[tmpres[j2], self.cres], writes=[tmpres[j2]])
            if kb > 0:
                cx.op("dve", lambda e: e.tensor_tensor(out=cs[:, c0:512], in0=CBk[:, c0:512], in1=cs[:, c0:512], op=ALU.add),
                      reads=[self.pres[bC], csres], writes=[csres])

        def s2b(t, step):
            i = t["i"]
            c0 = t["c0"]
            j = i % NB
            j2 = i % 2
            cx.op("act", lambda e: e.activation(out=pt[j][:, c0:512], in_=tmp[j2][:, c0:512], func=AF.Exp, scale=-1.0),
                  reads=[tmpres[j2]], writes=[ptres[j]])

        def s3(t, step):
            h, c, kb, nkb, i, u_ = t["h"], t["c"], t["kb"], t["nkb"], t["i"], t["u"]
            hb = h % 2
            c0 = t["c0"]
            j = i % NB
            bO = 6 + (u_ % 2)
            cx.op("pe", lambda e: e.matmul(self.ps[bO][:, c0:512], vt[:, kb, h * 128:(h + 1) * 128], pt[j][:, c0:512],
                                           start=(kb == nkb - 1), stop=(kb == 0)),
                  reads=[ptres[j], vres], writes=[self.pres[bO]])
            if kb == 0:
                def fin():
                    cx.op("act", lambda e: e.activation(out=oh[hb][:, c * 512:(c + 1) * 512], in_=self.ps[bO][:], func=AF.Copy),
                          reads=[self.pres[bO]], writes=[ohres[hb]])
                    if t["last_of_head"]:
                        cx.dma("sp", self.oT[1, h * 128:(h + 1) * 128, :], oh[hb][:], reads=[ohres[hb]])
                self.defer(step + 1, fin)

        self.run_pipeline(tasks, [s1, s2, s2b, s3], [0, 2, 3, 5])

    def phase_attn_c(self, l):
        cx = self.cx
        cx.barrier()
        cx.release(self.persist_mark)
        cf, cb = self.cf, self.cb
        ones = self.ones()
        ident = cb[:, CB_IDENT:CB_IDENT + 128]
        vt, vres = self.load_v(2)
        ltab = cx.sb([9, S], BF16, "ltab")
        ltres = Res("ltab")
        cx.dma("sp", ltab[:], self.c_rows[17:26, :], writes=[ltres])
        qk = [cx.sb([128, 2, S], BF16, "qkC") for _ in range(2)]
        qkres = [Res("qkC") for _ in range(2)]
        rt = [cx.sb([9, S], BF16, "rtC") for _ in range(2)]
        rtres = [Res("rtC") for _ in range(2)]
        oh = [cx.sb([128, S], BF16, "ohC") for _ in range(2)]
        ohres = [Res("ohC") for _ in range(2)]
        NP = 3
        pt = [cx.sb([128, 512], BF16, "ptC") for _ in range(NP)]
        ptres = [Res("ptC") for _ in range(NP)]
        km = cx.sb([128, 8], F32, "km")
        kh = cx.sb([128, 8], BF16, "kh")
        kl = cx.sb([128, 8], BF16, "kl")
        gm = cx.sb([128, 128], F32, "gm")
        mx = cx.sb([128, 128], F32, "mx")
        sel = cx.sb([128, 128], F32, "sel")
        mb = cx.sb([128, 128], BF16, "mb")
        gres = Res("gsel")
        rr = cx.sb([128, 512], F32, "rrC")
        rrres = Res("rrC")

        def load_head(h):
            hb = h % 2
            r0 = 6144 + h * 128
            cx.dma("sp", qk[hb][:, 0, :], self.projT[r0:r0 + 128, :], writes=[qkres[hb]])
            cx.dma("sp", qk[hb][:, 1, :], self.projT[r0 + 1024:r0 + 1024 + 128, :], writes=[qkres[hb]])
            cx.dma("sp", rt[hb][8:9, :], self.c_rows[9 + h:10 + h, :], writes=[rtres[hb]])

        def gate_head(h, gstep=None):
            hb = h % 2
            cx.op("dve", lambda e: e.tensor_reduce(out=km[:], in_=qk[hb][:, 1, :].rearrange("p (n k) -> p n k", k=256),
                                                   axis=AX.X, op=ALU.add),
                  reads=[qkres[hb]], writes=[gres])
            cx.op("dve", lambda e: e.tensor_copy(out=kh[:], in_=km[:]), reads=[gres], writes=[gres])
            cx.op("dve", lambda e: e.tensor_tensor(out=kl[:], in0=km[:], in1=kh[:], op=ALU.subtract), reads=[gres], writes=[gres])
            bG = 6

            def mmG(e):
                ins = None
                for qb in range(16):
                    e.matmul(self.ps[bG][:, qb * 8:(qb + 1) * 8], qk[hb][:, 0, qb * 128:(qb + 1) * 128], kh[:], start=True, stop=False)
                    ins = e.matmul(self.ps[bG][:, qb * 8:(qb + 1) * 8], qk[hb][:, 0, qb * 128:(qb + 1) * 128], kl[:], start=False, stop=True)
                return ins
            cx.op("pe", mmG, reads=[qkres[hb], gres], writes=[self.pres[bG]])
            cx.op("dve", lambda e: e.tensor_tensor(out=gm[:], in0=self.ps[bG][:, 0:128], in1=cf[:, CF_PAD:CF_PAD + 128], op=ALU.add),
                  reads=[self.pres[bG], self.cres], writes=[gres])
            for qb in range(16):
                cx.op("dve", lambda e, qb=qb: e.max(out=mx[:, qb * 8:(qb + 1) * 8], in_=gm[:, qb * 8:(qb + 1) * 8]),
                      reads=[gres], writes=[gres])
            for qb in range(16):
                cx.op("dve", lambda e, qb=qb: e.tensor_scalar(
                    out=sel[:, qb * 8:(qb + 1) * 8], in0=gm[:, qb * 8:(qb + 1) * 8], scalar1=mx[:, qb * 8 + 2:qb * 8 + 3],
                    scalar2=-NEG, op0=ALU.is_ge, op1=ALU.mult), reads=[gres], writes=[gres])
            cx.op("dve", lambda e: e.scalar_tensor_tensor(out=mb[:], in0=sel[:], scalar=NEG, in1=cf[:, CF_PAST:CF_PAST + 128],
                                                          op0=ALU.add, op1=ALU.mult),
                  reads=[gres, self.cres], writes=[gres])
            def transposes():
              for half in range(2):
                bT = 6 + half
                tb = self.ps[bT][:, :].bitcast(BF16)

                def mmT(e, half=half, tb=tb):
                    ins = None
                    for q8 in range(8):
                        qb = half * 8 + q8
                        ins = e.transpose(tb[0:8, q8 * 128:(q8 + 1) * 128], mb[:, qb * 8:(qb + 1) * 8], ident)
                    return ins
                cx.op("pe", mmT, reads=[gres, self.cres], writes=[self.pres[bT]])
                cx.op("act", lambda e, half=half, tb=tb: e.activation(
                    out=rt[hb][0:8, half * 1024:(half + 1) * 1024], in_=tb[0:8, :], func=AF.Copy),
                    reads=[self.pres[bT]], writes=[rtres[hb]])
            if gstep is None:
                transposes()
            else:
                self.defer(gstep + 14, transposes)

        tasks = []
        u = 0
        i = 0
        for h in range(8):
            for c in range(4):
                nkb = 4 * c + 4
                for kb in range(nkb):
                    tasks.append(dict(h=h, c=c, kb=kb, nkb=nkb, u=u, i=i, first_of_head=(c == 0 and kb == 0),
                                      mid_of_head=(c == 2 and kb == 0), last_of_head=(c == 3 and kb == nkb - 1)))
                    i += 1
                u += 1
        load_head(0)
        gate_head(0)

        def s1(t, step):
            h, c, kb, i = t["h"], t["c"], t["kb"], t["i"]
            hb = h % 2
            if t["first_of_head"] and h + 1 < 8:
                load_head(h + 1)
            if t["mid_of_head"] and h + 1 < 8:
                gate_head(h + 1, step)
            d = kb - 4 * c
            c0 = t["c0"] = 128 * d if d > 0 else 0
            bS = i % 2
            S_ = self.ps[bS]
            p_ = i % NP
            qs = slice(c * 512 + c0, (c + 1) * 512)
            ks = slice(kb * 128, (kb + 1) * 128)

            def mmS(e):
                e.matmul(S_[:, c0:512], qk[hb][:, 1, ks], qk[hb][:, 0, qs], start=True, stop=False)
                return e.matmul(S_[:, c0:512], ltab[0:9, ks], rt[hb][0:9, qs], start=False, stop=True)
            cx.op("pe", mmS, reads=[qkres[hb], rtres[hb], ltres], writes=[self.pres[bS]])
            if d >= 0:
                cx.op("dve", lambda e: e.tensor_tensor(
                    out=S_[:, 128 * d:128 * d + 128], in0=S_[:, 128 * d:128 * d + 128],
                    in1=cf[:, CF_TRINEG:CF_TRINEG + 128], op=ALU.add),
                    reads=[self.pres[bS], self.cres], writes=[self.pres[bS]])
            bcol = CF_KPC + h * 16 + kb
            cx.op("act", lambda e: e.activation(out=pt[p_][:, c0:512], in_=S_[:, c0:512], func=AF.Exp,
                                                bias=cf[:, bcol:bcol + 1], scale=1.0),
                  reads=[self.pres[bS], self.cres], writes=[ptres[p_]])

        def s2(t, step):
            h, c, kb, nkb, i, u_ = t["h"], t["c"], t["kb"], t["nkb"], t["i"], t["u"]
            hb = h % 2
            c0 = t["c0"]
            p_ = i % NP
            bO = 2 + 2 * (u_ % 2)
            bSm = bO + 1

            def mmO(e):
                e.matmul(self.ps[bO][:, c0:512], vt[:, kb, h * 128:(h + 1) * 128], pt[p_][:, c0:512],
                         start=(kb == 0), stop=(kb == nkb - 1))
                return e.matmul(self.ps[bSm][:, c0:512], ones, pt[p_][:, c0:512], start=(kb == 0), stop=(kb == nkb - 1))
            cx.op("pe", mmO, reads=[ptres[p_], vres, self.cres], writes=[self.pres[bO], self.pres[bSm]])
            if kb == nkb - 1:
                def fin():
                    cx.op("act", lambda e: e.activation(out=rr[:], in_=self.ps[bSm][:], func=AF.Ln), reads=[self.pres[bSm]], writes=[rrres])
                    cx.op("act", lambda e: e.activation(out=rr[:], in_=rr[:], func=AF.Exp, scale=-1.0), reads=[rrres], writes=[rrres])
                    cx.op("dve", lambda e: e.tensor_tensor(out=oh[hb][:, c * 512:(c + 1) * 512], in0=self.ps[bO][:], in1=rr[:],
                                                           op=ALU.mult),
                          reads=[self.pres[bO], rrres], writes=[ohres[hb]])
                    if t["last_of_head"]:
                        cx.dma("sp", self.oT[2, h * 128:(h + 1) * 128, :], oh[hb][:], reads=[ohres[hb]])
                self.defer(step + 1, fin)

        self.run_pipeline(tasks, [s1, s2], [0, 1])

    def phase_merge(self, l):
        cx = self.cx
        cx.barrier()
        cx.release(self.persist_mark)
        oall = cx.sb([128, 24, S], BF16, "oall")
        ores = [Res("oall%d" % i) for i in range(3)]
        for i in range(3):
            cx.dma("pool", oall[:, i * 8:(i + 1) * 8, :], self.oT[i].rearrange("(hc p) s -> p hc s", p=128), writes=[ores[i]], phase_local=True)
        gsb = [cx.sb([128, 3, S], BF16, "gsb") for _ in range(2)]
        gsres = [Res("gsb") for _ in range(2)]
        mst = [cx.sb([128, S], BF16, "mst") for _ in range(2)]
        mstres = [Res("mst") for _ in range(2)]
        ta = cx.sb([128, 512], F32, "ta")
        tbb = cx.sb([128, 512], F32, "tb")
        tares, tbres = Res("ta"), Res("tb")
        wv = self.w_branch[l].rearrange("i (hc p) n -> p i hc n", p=128)
        bi = 0
        for cg in range(4):
            c0 = cg * 512
            s = self.next_slot()
            slot = self.wslot[s][:, 0:12288].rearrange("p (i hc n) -> p i hc n", i=3, hc=8)
            for i in range(3):
                cx.dma("pool", slot[:, i, :, :], wv[:, i, :, c0:c0 + 512], writes=[self.wres[s]])
            for sub in range(4):
                b = bi % 2
                bi += 1
                dblk = cg * 4 + sub
                for i in range(3):
                    cx.dma("sp", gsb[b][:, i, :], self.gateT[i * 2048 + dblk * 128:i * 2048 + (dblk + 1) * 128, :],
                           writes=[gsres[b]])
                for tg in range(4):
                    zb = []
                    for i in range(3):
                        pst, pr = self.bank()
                        zb.append((pst, pr))

                        def mm(e, i=i, pst=pst, slot=slot, sub=sub, tg=tg):
                            ins = None
                            for hc in range(8):
                                ins = e.matmul(pst[:], slot[:, i, hc, sub * 128:(sub + 1) * 128],
                                               oall[:, i * 8 + hc, tg * 512:(tg + 1) * 512], start=(hc == 0), stop=(hc == 7))
                            return ins
                        cx.op("pe", mm, reads=[self.wres[s], ores[i]], writes=[pr])
                    ts = slice(tg * 512, (tg + 1) * 512)
                    cx.op("dve", lambda e, b=b, ts=ts, z=zb[0][0]: e.tensor_tensor(out=ta[:], in0=z[:], in1=gsb[b][:, 0, ts], op=ALU.mult),
                          reads=[zb[0][1], gsres[b]], writes=[tares])
                    cx.op("dve", lambda e, b=b, ts=ts, z=zb[1][0]: e.tensor_tensor(out=tbb[:], in0=z[:], in1=gsb[b][:, 1, ts], op=ALU.mult),
                          reads=[zb[1][1], gsres[b]], writes=[tbres])
                    cx.op("dve", lambda e: e.tensor_tensor(out=ta[:], in0=ta[:], in1=tbb[:], op=ALU.add),
                          reads=[tares, tbres], writes=[tares])
                    cx.op("dve", lambda e, b=b, ts=ts, z=zb[2][0]: e.tensor_tensor(out=tbb[:], in0=z[:], in1=gsb[b][:, 2, ts], op=ALU.mult),
                          reads=[zb[2][1], gsres[b]], writes=[tbres])
                    cx.op("dve", lambda e, b=b, ts=ts: e.tensor_tensor(out=mst[b][:, ts], in0=ta[:], in1=tbb[:], op=ALU.add),
                          reads=[tares, tbres], writes=[mstres[b]])
                cx.dma("sp", self.mergedT[dblk * 128:(dblk + 1) * 128, :], mst[b][:], reads=[mstres[b]])

    def residual_gemm(self, xsrc, w_ap, a_dram, nkc, npass=1):
        cx = self.cx
        cx.barrier()
        cx.release(self.persist_mark)
        nb = 1 if npass == 1 else 2
        aT = [cx.sb([128, nkc, S], BF16, "aT") for _ in range(nb)]
        ares = [Res("aT") for _ in range(nb)]
        xr = [cx.sb([128, S], F32, "xr") for _ in range(3)]
        xrres = [Res("xr") for _ in range(3)]
        rows = nkc * 128

        def load_a(p):
            cx.dma("pool", aT[p % nb][:], a_dram[p * rows:(p + 1) * rows, :].rearrange("(kc p) s -> p kc s", p=128),
                   writes=[ares[p % nb]], phase_local=(p < 2))
        load_a(0)
        xi = 0
        for p in range(npass):
            wv = w_ap[p * rows:(p + 1) * rows, :].rearrange("(kc p) n -> p kc n", p=128)
            src_x = xsrc if p == 0 else self.xres
            at = aT[p % nb]
            for cg in range(4):
                c0 = cg * 512
                s = self.next_slot()
                slot = self.wslot[s][:, 0:nkc * 512].rearrange("p (kc n) -> p kc n", kc=nkc)
                cx.dma("pool", slot, wv[:, :, c0:c0 + 512], writes=[self.wres[s]])
                if cg == 0 and p + 1 < npass:
                    load_a(p + 1)
                for sub in range(4):
                    blk = cg * 4 + sub
                    b = xi % 3
                    xi += 1
                    cx.dma("sp", xr[b][:], src_x[blk * 128:(blk + 1) * 128, :], reads=[self.xres_r[blk]], writes=[xrres[b]])
                    for tg in range(4):
                        pst, pr = self.bank()

                        def mm(e, pst=pst, slot=slot, sub=sub, tg=tg, at=at):
                            ins = None
                            for kc in range(nkc):
                                ins = e.matmul(pst[:], slot[:, kc, sub * 128:(sub + 1) * 128], at[:, kc, tg * 512:(tg + 1) * 512],
                                               start=(kc == 0), stop=(kc == nkc - 1))
                            return ins
                        cx.op("pe", mm, reads=[self.wres[s], ares[p % nb]], writes=[pr])
                        ts = slice(tg * 512, (tg + 1) * 512)
                        cx.op("dve", lambda e, b=b, ts=ts, pst=pst: e.tensor_tensor(out=xr[b][:, ts], in0=pst[:], in1=xr[b][:, ts], op=ALU.add),
                              reads=[pr, xrres[b]], writes=[xrres[b]])
                    cx.dma("sp", self.xres[blk * 128:(blk + 1) * 128, :], xr[b][:], reads=[xrres[b]], writes=[self.xres_r[blk]])

    def phase_ffn_up(self, l):
        cx = self.cx
        cx.barrier()
        cx.release(self.persist_mark)
        hT = cx.sb([128, 16, S], BF16, "hT2")
        hres = [Res("hT2_%d" % i) for i in range(16)]
        m = self.rmsnorm_to_sbuf(self.xres, self.g_ffn[l], hT, hres)
        cx.release(m)
        ast = [cx.sb([128, S], BF16, "ast") for _ in range(2)]
        astres = [Res("ast") for _ in range(2)]
        sg = [cx.sb([128, 512], F32, "sg") for _ in range(2)]
        sgres = [Res("sg") for _ in range(2)]
        wg = self.w_gate[l].rearrange("(kc p) n -> p kc n", p=128)
        wu = self.w_up[l].rearrange("(kc p) n -> p kc n", p=128)
        ai = 0
        si = 0
        for fg in range(22):
            f0 = fg * 256
            s = self.next_slot()
            slot = self.wslot[s][:, 0:8192].rearrange("p (g kc n) -> p g kc n", g=2, kc=16)
            cx.dma("pool", slot[:, 0, :, :], wg[:, :, f0:f0 + 256], writes=[self.wres[s]])
            cx.dma("pool", slot[:, 1, :, :], wu[:, :, f0:f0 + 256], writes=[self.wres[s]])
            for sub in range(2):
                b = ai % 2
                ai += 1
                fblk = fg * 2 + sub
                for tg in range(4):
                    pg, prg = self.bank()
                    pu, pru = self.bank()

                    def mm(e, pg=pg, pu=pu, slot=slot, sub=sub, tg=tg):
                        ins = None
                        for kc in range(16):
                            e.matmul(pg[:], slot[:, 0, kc, sub * 128:(sub + 1) * 128], hT[:, kc, tg * 512:(tg + 1) * 512],
                                     start=(kc == 0), stop=(kc == 15))
                        for kc in range(16):
                            ins = e.matmul(pu[:], slot[:, 1, kc, sub * 128:(sub + 1) * 128], hT[:, kc, tg * 512:(tg + 1) * 512],
                                           start=(kc == 0), stop=(kc == 15))
                        return ins
                    cx.op("pe", mm, reads=[self.wres[s]] + hres, writes=[prg, pru])
                    j = si % 2
                    si += 1
                    cx.op("act", lambda e, j=j, pg=pg: e.activation(out=sg[j][:], in_=pg[:], func=AF.Silu),
                          reads=[prg], writes=[sgres[j]])
                    ts = slice(tg * 512, (tg + 1) * 512)
                    cx.op("dve", lambda e, j=j, b=b, ts=ts, pu=pu: e.tensor_tensor(out=ast[b][:, ts], in0=pu[:], in1=sg[j][:], op=ALU.mult),
                          reads=[pru, sgres[j]], writes=[astres[b]])
                cx.dma("sp", self.actT[fblk * 128:(fblk + 1) * 128, :], ast[b][:], reads=[astres[b]])

    def phase_final(self, xsrc):
        cx = self.cx
        cx.barrier()
        cx.release(self.persist_mark)
        self.rmsnorm_to_sbuf(xsrc, self.g_fin, None, None, out_dram=self.outT)


CB_ONES = 0
CB_TRI = 128
CB_IDENT = 256
CB_M01S = 384
CB_COLS = 512
CF_EPS = 0
CF_ONE = 1
CF_KPA = 8
CF_KPC = 136
CF_TRINEG = 264
CF_TRIPOS = 392
CF_PAD = 520
CF_PAST = 648
CF_COLS = 776
NROWS = 26


def make_consts():
    p = np.arange(128)
    cb = np.zeros((128, CB_COLS), dtype=np.float32)
    cb[:, CB_ONES:CB_ONES + 128] = 1.0
    cb[:, CB_TRI:CB_TRI + 128] = (p[:, None] >= p[None, :])
    cb[:, CB_IDENT:CB_IDENT + 128] = np.eye(128)
    cb[:, CB_M01S:CB_M01S + 128] = (p[:, None] < p[None, :])
    cf = np.zeros((128, CF_COLS), dtype=np.float32)
    cf[:, CF_EPS] = EPS
    cf[:, CF_ONE] = 1.0
    for h in range(8):
        for kb in range(16):
            cf[:, CF_KPA + h * 16 + kb] = ALIBI_DIFF[h] * (kb * 128 + p)
            cf[:, CF_KPC + h * 16 + kb] = ALIBI_MOBA[h] * (kb * 128 + p)
    cf[:, CF_TRINEG:CF_TRINEG + 128] = np.where(p[:, None] <= p[None, :], 0.0, NEG)
    cf[:, CF_TRIPOS:CF_TRIPOS + 128] = np.where(p[:, None] < p[None, :], 0.0, -NEG)
    for qb in range(16):
        own = qb // 2
        for n in range(8):
            cf[:, CF_PAD + qb * 8 + n] = 0.0 if n < own else -3.0e38
            cf[:, CF_PAST + qb * 8 + n] = 1.0 if n < own else 0.0
    t = np.arange(S, dtype=np.float64)
    rows = np.zeros((NROWS, S), dtype=np.float32)
    for h in range(8):
        rows[h] = -ALIBI_DIFF[h] * t
        rows[9 + h] = -ALIBI_MOBA[h] * t
    rows[8] = 1.0
    for n in range(8):
        rows[17 + n] = (np.arange(S) // 256 == n)
    rows[25] = 1.0
    return cb.astype(ml_dtypes.bfloat16), cf, rows.astype(ml_dtypes.bfloat16)


def make_inputs(inp, b):
    cb, cf, rows = make_consts()
    f = lambda a: np.ascontiguousarray(a, dtype=np.float32)
    d = {
        "xT": f(inp["x"][b].T),
        "w_in": f(inp["w_in"]), "w_branch": f(inp["w_branch"]), "w_out": f(inp["w_out"]),
        "w_ffn_gate": f(inp["w_ffn_gate"]), "w_ffn_up": f(inp["w_ffn_up"]), "w_ffn_down": f(inp["w_ffn_down"]),
        "g_mix": f(inp["norm_mix_g"].reshape(DEPTH, 16, 128).transpose(0, 2, 1)),
        "g_ffn": f(inp["norm_ffn_g"].reshape(DEPTH, 16, 128).transpose(0, 2, 1)),
        "g_fin": f(inp["final_norm_g"].reshape(16, 128).T),
        "b_gate": f(inp["b_gate"].reshape(DEPTH, 48, 128).transpose(0, 2, 1)),
        "lamv": f(np.stack([inp["lam_q1"], inp["lam_k1"], inp["lam_q2"], inp["lam_k2"]], axis=1)),
        "subln": f(inp["subln_w"].reshape(DEPTH, 128, 1)),
        "c_bf": cb, "c_f32": cf, "c_rows": rows,
    }
    return d


def kernel(**inputs):
    prog = Prog()
    n = 8
    shared = None
    in_maps = []
    for b in range(n):
        d = make_inputs(inputs, b) if shared is None else dict(shared, xT=np.ascontiguousarray(inputs["x"][b].T, dtype=np.float32))
        if shared is None:
            shared = d
        in_maps.append(d)
    res = run_bass_kernel_spmd(prog.nc, in_maps, core_ids=list(range(n)))
    out = np.stack([np.ascontiguousarray(r["outT"].T) for r in res.results], axis=0)
    return out.astype(np.float32)
```

```python
import math
import numpy as np
import ml_dtypes
import concourse.bass as bass
import concourse.mybir as mybir
from concourse.bass_utils import run_bass_kernel_spmd

F32 = mybir.dt.float32
BF16 = mybir.dt.bfloat16
AF = mybir.ActivationFunctionType
ALU = mybir.AluOpType
AX = mybir.AxisListType

D = 2048
S = 2048
DEPTH = 2
NCOLS = 15360
DFF = 5632
EPS = 1e-6
NEG = -30000.0
N_ALIBI = 16
ALIBI_ALL = 2.0 ** (-8.0 * (np.arange(N_ALIBI) + 1) / N_ALIBI)
ALIBI_DIFF = ALIBI_ALL[0::2].astype(np.float32)
ALIBI_MOBA = ALIBI_ALL[1::2].astype(np.float32)

SB_BASE = 16512
SB_TOP = 229344


class Res:
    __slots__ = ("name", "w", "r")

    def __init__(self, name=""):
        self.name = name
        self.w = None
        self.r = []


class Stream:
    def __init__(self, name):
        self.name = name
        self.items = []
        self.count = 0
        self.sem = None
        self.waited = {}
        self.ring = []
        self.ring_pos = 0


class Ctx:
    COMPUTE = ("pe", "act", "dve", "pool")

    def __init__(self, nc):
        self.nc = nc
        self.st = {n: Stream(n) for n in ("pe", "act", "dve", "pool", "sp")}
        for n in self.COMPUTE:
            self.st[n].sem = nc.alloc_semaphore("c_" + n)
        for n, k in (("sp", 4), ("pool", 4), ("act", 2)):
            for i in range(k):
                self.st[n].ring.append([nc.alloc_semaphore("d_%s%d" % (n, i)), 0, None])
        self.sb_off = SB_BASE
        self.last_barrier = []
        self.nalloc = 0
        self.bank = 0

    def sb(self, shape, dtype, name="t"):
        nbytes = int(np.prod(shape[1:])) * (4 if dtype == F32 else 2)
        nbytes = (nbytes + 31) // 32 * 32
        assert self.sb_off + nbytes <= SB_TOP, ("sbuf overflow", name, self.sb_off, nbytes)
        self.nalloc += 1
        t = self.nc.alloc_sbuf_tensor_at("%s_%d" % (name, self.nalloc), list(shape), dtype, offset=self.sb_off)
        self.sb_off += nbytes
        return t

    def mark(self):
        return self.sb_off

    def release(self, m):
        self.sb_off = m

    def _need(self, st, tok, waits):
        if tok is None:
            return
        kind, key, val, sem = tok
        if kind == "c" and key == "pe" and st.name == "pe":
            return
        k = (kind, key)
        if st.waited.get(k, 0) >= val:
            return
        st.waited[k] = val
        waits.append((sem, val))

    def _collect(self, st, reads, writes):
        waits = []
        for r in reads:
            self._need(st, r.w, waits)
        for w in writes:
            self._need(st, w.w, waits)
            for t in w.r:
                self._need(st, t, waits)
        return waits

    def _update(self, tok, reads, writes):
        for r in reads:
            r.r.append(tok)
        for w in writes:
            w.w = tok
            w.r = []

    def op(self, eng, fn, reads=(), writes=()):
        st = self.st[eng]
        waits = self._collect(st, reads, writes)
        st.count += 1
        tok = ("c", eng, st.count, st.sem)
        st.items.append((waits, fn, (st.sem, 1)))
        self._update(tok, reads, writes)
        return tok

    def dma(self, q, out, in_, reads=(), writes=(), phase_local=False):
        st = self.st[q]
        waits = self._collect(st, reads, writes)
        if phase_local:
            for t in self.last_barrier:
                if not (t[0] == "c" and t[1] == q):
                    self._need(st, t, waits)
        slot = st.ring[st.ring_pos % len(st.ring)]
        st.ring_pos += 1
        self._need(st, slot[2], waits)
        slot[1] += 16
        tok = ("d", slot[0].num, slot[1], slot[0])
        slot[2] = tok
        st.items.append((waits, (lambda e, o=out, i=in_: e.dma_start(out=o, in_=i)), (slot[0], 16)))
        self._update(tok, reads, writes)
        return tok

    def barrier(self, engines=("pe", "act", "dve", "sp")):
        toks = []
        for n in self.COMPUTE:
            s = self.st[n]
            if s.count:
                toks.append(("c", n, s.count, s.sem))
        for n in ("sp", "pool", "act"):
            for slot in self.st[n].ring:
                if slot[2] is not None:
                    toks.append(slot[2])
        self.last_barrier = toks
        for n in engines:
            st = self.st[n]
            waits = []
            for t in toks:
                if t[0] == "c" and t[1] == n:
                    continue
                self._need(st, t, waits)
            if waits:
                st.items.append((waits, None, None))

    def next_bank(self):
        b = self.bank
        self.bank = (self.bank + 1) % 8
        return b

    def emit(self):
        nc = self.nc
        st = self.st

        def run(s, e):
            for waits, fn, inc in s.items:
                for sem, val in waits:
                    e.wait_ge(sem, val)
                if fn is not None:
                    ins = fn(e)
                    ins.then_inc(inc[0], inc[1])

        with nc.Block() as block:
            @block.tensor
            def _(e):
                run(st["pe"], e)

            @block.scalar
            def _(e):
                run(st["act"], e)

            @block.vector
            def _(e):
                run(st["dve"], e)

            @block.gpsimd
            def _(e):
                run(st["pool"], e)

            @block.sync
            def _(e):
                run(st["sp"], e)


class Prog:
    def __init__(self, n_layers=DEPTH, stop_after=None, taps=(), skip=()):
        self.skip = set(skip)
        self.n_layers = n_layers
        self.stop_after = stop_after
        self.taps = set(taps)
        nc = self.nc = bass.Bass("TRN2", target_bir_lowering=False)
        self.cx = Ctx(nc)
        ein = lambda name, shape, dt=F32: nc.dram_tensor(name, list(shape), dt, kind="ExternalInput").ap()
        self.xT = ein("xT", [D, S])
        self.w_in = ein("w_in", [DEPTH, D, NCOLS])
        self.w_branch = ein("w_branch", [DEPTH, 3, 1024, D])
        self.w_out = ein("w_out", [DEPTH, D, D])
        self.w_gate = ein("w_ffn_gate", [DEPTH, D, DFF])
        self.w_up = ein("w_ffn_up", [DEPTH, D, DFF])
        self.w_down = ein("w_ffn_down", [DEPTH, DFF, D])
        self.g_mix = ein("g_mix", [DEPTH, 128, 16])
        self.g_ffn = ein("g_ffn", [DEPTH, 128, 16])
        self.g_fin = ein("g_fin", [128, 16])
        self.b_gate = ein("b_gate", [DEPTH, 128, 48])
        self.lamv = ein("lamv", [DEPTH, 4, 64])
        self.subln = ein("subln", [DEPTH, 128, 1])
        self.c_bf = ein("c_bf", [128, CB_COLS], BF16)
        self.c_f32 = ein("c_f32", [128, CF_COLS])
        self.c_rows = ein("c_rows", [NROWS, S], BF16)
        self.outT = nc.dram_tensor("outT", [D, S], F32, kind="ExternalOutput").ap()

        def scratch(name, shape, dt):
            kind = "ExternalOutput" if name in self.taps else "Internal"
            return nc.dram_tensor(name, list(shape), dt, kind=kind).ap()
        self.xres = scratch("xres", [D, S], F32)
        self.projT = scratch("projT", [9216, S], BF16)
        self.vtok = scratch("vtok", [3, S, 1024], BF16)
        self.gateT = scratch("gateT", [6144, S], BF16)
        self.oT = scratch("oT", [3, 1024, S], BF16)
        self.mergedT = scratch("mergedT", [D, S], BF16)
        self.actT = scratch("actT", [DFF, S], BF16)
        self.hdbg = scratch("hdbg", [D, S], BF16)

        cx = self.cx
        self.cb = cx.sb([128, CB_COLS], BF16, "cb")
        self.cf = cx.sb([128, CF_COLS], F32, "cf")
        self.cres = Res("consts")
        cx.dma("sp", self.cb[:], self.c_bf[:, :], writes=[self.cres])
        cx.dma("sp", self.cf[:], self.c_f32[:, :], writes=[self.cres])
        self.NSLOT = 2
        self.wslot = [cx.sb([128, 12288], BF16, "wslot") for _ in range(self.NSLOT)]
        self.wres = [Res("wslot%d" % i) for i in range(self.NSLOT)]
        self.wpos = 0
        self.ps = [nc.alloc_psum_tensor("ps%d" % i, [128, 512], F32) for i in range(8)]
        self.pres = [Res("ps%d" % i) for i in range(8)]
        self.xres_r = [Res("xres%d" % i) for i in range(16)]
        self.persist_mark = cx.mark()

        self.build()
        cx.barrier(engines=("pe", "act", "dve", "pool", "sp"))
        cx.emit()

    def ones(self):
        return self.cb[:, CB_ONES:CB_ONES + 128]

    def next_slot(self):
        s = self.wpos % self.NSLOT
        self.wpos += 1
        return s

    def bank(self):
        b = self.cx.next_bank()
        return self.ps[b], self.pres[b]

    def build(self):
        xsrc = self.xT
        for l in range(self.n_layers):
            steps = [
                ("proj", lambda: self.phase_proj(l, xsrc)),
                ("attn_a", lambda: self.phase_attn_a(l)),
                ("attn_b", lambda: self.phase_attn_b(l)),
                ("attn_c", lambda: self.phase_attn_c(l)),
                ("merge", lambda: self.phase_merge(l)),
                ("wout", lambda: self.residual_gemm(xsrc, self.w_out[l], self.mergedT, 16)),
                ("ffn_up", lambda: self.phase_ffn_up(l)),
            ]
            for name, fn in steps:
                if name in self.skip:
                    continue
                fn()
                if self.stop_after == (name, l):
                    return
            self.residual_gemm(self.xres, self.w_down[l], self.actT, 11, npass=4)
            if self.stop_after == ("ffn_down", l):
                return
            xsrc = self.xres
        self.phase_final(xsrc)

    def rmsnorm_to_sbuf(self, xsrc, g_ap, hT, hres, out_dram=None):
        cx = self.cx
        m = cx.mark()
        gt = cx.sb([128, 16], F32, "g")
        gres = Res("g")
        cx.dma("sp", gt[:], g_ap, writes=[gres])
        xin = [cx.sb([128, S], F32, "xin") for _ in range(2)]
        xin_r = [Res("xin") for _ in range(2)]
        sq = [cx.sb([128, S], BF16, "sq") for _ in range(2)]
        sq_r = [Res("sq") for _ in range(2)]
        rstd = cx.sb([128, S], F32, "rstd")
        rstd_r = Res("rstd")
        banks = [self.bank() for _ in range(4)]
        ones = self.ones()
        for kc in range(16):
            b = kc % 2
            cx.dma("sp", xin[b][:], xsrc[kc * 128:(kc + 1) * 128, :], reads=[self.xres_r[kc]], writes=[xin_r[b]])
            cx.op("act", lambda e, b=b: e.activation(out=sq[b][:], in_=xin[b][:], func=AF.Square),
                  reads=[xin_r[b]], writes=[sq_r[b]])

            def mm(e, b=b, kc=kc):
                ins = None
                for tg in range(4):
                    ins = e.matmul(banks[tg][0][:], ones, sq[b][:, tg * 512:(tg + 1) * 512],
                                   start=(kc == 0), stop=(kc == 15))
                return ins
            cx.op("pe", mm, reads=[sq_r[b], self.cres], writes=[bk[1] for bk in banks])
            if out_dram is None:
                cx.op("dve", lambda e, b=b, kc=kc: e.tensor_copy(out=hT[:, kc, :], in_=xin[b][:]),
                      reads=[xin_r[b]], writes=[hres[kc]])
        for tg in range(4):
            cx.op("act", lambda e, tg=tg: e.activation(out=rstd[:, tg * 512:(tg + 1) * 512], in_=banks[tg][0][:],
                                                        func=AF.Ln, bias=self.cf[:, CF_EPS:CF_EPS + 1], scale=1.0 / D),
                  reads=[banks[tg][1], self.cres], writes=[rstd_r])
        cx.op("act", lambda e: e.activation(out=rstd[:], in_=rstd[:], func=AF.Exp, scale=-0.5), reads=[rstd_r], writes=[rstd_r])
        for kc in range(16):
            b = kc % 2
            if out_dram is None:
                cx.op("dve", lambda e, kc=kc: e.scalar_tensor_tensor(
                    out=hT[:, kc, :], in0=hT[:, kc, :], scalar=gt[:, kc:kc + 1], in1=rstd[:], op0=ALU.mult, op1=ALU.mult),
                    reads=[rstd_r, gres], writes=[hres[kc]])
            else:
                cx.dma("sp", xin[b][:], xsrc[kc * 128:(kc + 1) * 128, :], reads=[self.xres_r[kc]], writes=[xin_r[b]])
                cx.op("dve", lambda e, b=b, kc=kc: e.scalar_tensor_tensor(
                    out=xin[b][:], in0=xin[b][:], scalar=gt[:, kc:kc + 1], in1=rstd[:], op0=ALU.mult, op1=ALU.mult),
                    reads=[xin_r[b], rstd_r, gres], writes=[xin_r[b]])
                cx.dma("sp", out_dram[kc * 128:(kc + 1) * 128, :], xin[b][:], reads=[xin_r[b]])
        return m

    def phase_proj(self, l, xsrc):
        cx = self.cx
        cx.barrier()
        cx.release(self.persist_mark)
        hT = cx.sb([128, 16, S], BF16, "hT")
        hres = [Res("hT%d" % i) for i in range(16)]
        bg = cx.sb([128, 48], F32, "bg")
        bgres = Res("bg")
        cx.dma("sp", bg[:], self.b_gate[l], writes=[bgres])
        m = self.rmsnorm_to_sbuf(xsrc, self.g_mix[l], hT, hres)
        if "hdbg" in self.taps:
            for kc in range(16):
                cx.dma("sp", self.hdbg[kc * 128:(kc + 1) * 128, :], hT[:, kc, :], reads=[hres[kc]])
        cx.release(m)
        ost = [cx.sb([128, S], BF16, "ost") for _ in range(3)]
        ost_r = [Res("ost") for _ in range(3)]
        vst = [cx.sb([128, 512], BF16, "vst") for _ in range(3)]
        vst_r = [Res("vst") for _ in range(3)]
        opos = 0
        vpos = 0
        wv = self.w_in[l].rearrange("(kc p) n -> p kc n", p=128)
        for cg in range(30):
            c0 = cg * 512
            s = self.next_slot()
            slot = self.wslot[s][:, 0:8192].rearrange("p (kc n) -> p kc n", kc=16)
            cx.dma("pool", slot, wv[:, :, c0:c0 + 512], writes=[self.wres[s]])
            kind = "g" if cg >= 18 else ("q", "q", "k", "k", "v", "v")[cg % 6]
            br = (cg // 6) if cg < 18 else None
            if kind == "v":
                vcol0 = (cg % 6 - 4) * 512
                for tb in range(16):
                    pst, pr = self.bank()

                    def mm(e, tb=tb, pst=pst, slot=slot):
                        ins = None
                        for kc in range(16):
                            ins = e.matmul(pst[:], hT[:, kc, tb * 128:(tb + 1) * 128], slot[:, kc, :],
                                           start=(kc == 0), stop=(kc == 15))
                        return ins
                    cx.op("pe", mm, reads=[self.wres[s]] + hres, writes=[pr])
                    vb = vpos % 3
                    vpos += 1
                    cx.op("dve", lambda e, vb=vb, pst=pst: e.tensor_copy(out=vst[vb][:], in_=pst[:]),
                          reads=[pr], writes=[vst_r[vb]])
                    cx.dma("sp", self.vtok[br, tb * 128:(tb + 1) * 128, vcol0:vcol0 + 512], vst[vb][:],
                           reads=[vst_r[vb]])
                continue
            for sub in range(4):
                ob = opos % 3
                opos += 1
                col = c0 + sub * 128
                for tg in range(4):
                    pst, pr = self.bank()

                    def mm(e, sub=sub, tg=tg, pst=pst, slot=slot):
                        ins = None
                        for kc in range(16):
                            ins = e.matmul(pst[:], slot[:, kc, sub * 128:(sub + 1) * 128],
                                           hT[:, kc, tg * 512:(tg + 1) * 512], start=(kc == 0), stop=(kc == 15))
                        return ins
                    cx.op("pe", mm, reads=[self.wres[s]] + hres, writes=[pr])
                    dst = ost[ob][:, tg * 512:(tg + 1) * 512]
                    if kind == "g":
                        j = (col - 9216) // 128
                        cx.op("act", lambda e, dst=dst, pst=pst, j=j: e.activation(
                            out=dst, in_=pst[:], func=AF.Sigmoid, bias=bg[:, j:j + 1], scale=1.0),
                            reads=[pr, bgres], writes=[ost_r[ob]])
                    elif kind == "q":
                        sc = 0.125 if br == 0 else 128.0 ** -0.5
                        cx.op("dve", lambda e, dst=dst, pst=pst, sc=sc: e.tensor_scalar(
                            out=dst, in0=pst[:], scalar1=sc, scalar2=None, op0=ALU.mult),
                            reads=[pr], writes=[ost_r[ob]])
                    else:
                        cx.op("dve", lambda e, dst=dst, pst=pst: e.tensor_copy(out=dst, in_=pst[:]),
                              reads=[pr], writes=[ost_r[ob]])
                if kind == "g":
                    cx.dma("sp", self.gateT[col - 9216:col - 9216 + 128, :], ost[ob][:], reads=[ost_r[ob]])
                else:
                    cx.dma("sp", self.projT[col:col + 128, :], ost[ob][:], reads=[ost_r[ob]])


    def load_v(self, br):
        cx = self.cx
        vt = cx.sb([128, 16, 1024], BF16, "vt")
        vres = Res("vt")
        src = self.vtok[br].rearrange("(blk p) c -> p blk c", p=128)
        cx.dma("pool", vt[:], src, writes=[vres], phase_local=True)
        return vt, vres

    def run_pipeline(self, tasks, stages, skews):
        n = len(tasks)
        self.deferred = []
        maxs = max(skews)
        for step in range(n + maxs + 8):
            for st, sk in zip(stages, skews):
                i = step - sk
                if 0 <= i < n:
                    st(tasks[i], step)
            keep = []
            for due, fn in self.deferred:
                if due <= step:
                    fn()
                else:
                    keep.append((due, fn))
            self.deferred = keep
        assert not self.deferred

    def defer(self, due, fn):
        self.deferred.append((due, fn))

    def phase_attn_a(self, l):
        cx = self.cx
        cx.barrier()
        cx.release(self.persist_mark)
        lam_init = 0.8 - 0.6 * math.exp(-0.3 * l)
        cf, cb = self.cf, self.cb
        ones = self.ones()
        lv = cx.sb([128, 4, 64], F32, "lv")
        prod = cx.sb([128, 2, 64], F32, "prod")
        sm = cx.sb([128, 8], F32, "sm")
        sw = cx.sb([128, 1], F32, "sw")
        lres = Res("lam")
        cx.dma("sp", lv[:], self.lamv[l].partition_broadcast(128), writes=[lres])
        cx.dma("sp", sw[:], self.subln[l], writes=[lres])
        cx.op("dve", lambda e: e.tensor_tensor(out=prod[:, 0, :], in0=lv[:, 0, :], in1=lv[:, 1, :], op=ALU.mult),
              reads=[lres], writes=[lres])
        cx.op("dve", lambda e: e.tensor_tensor(out=prod[:, 1, :], in0=lv[:, 2, :], in1=lv[:, 3, :], op=ALU.mult),
              reads=[lres], writes=[lres])
        cx.op("dve", lambda e: e.reduce_sum(out=sm[:, 0:2], in_=prod[:], axis=AX.X), reads=[lres], writes=[lres])
        cx.op("act", lambda e: e.activation(out=sm[:, 2:4], in_=sm[:, 0:2], func=AF.Exp), reads=[lres], writes=[lres])
        cx.op("dve", lambda e: e.tensor_tensor(out=sm[:, 4:5], in0=sm[:, 3:4], in1=sm[:, 2:3], op=ALU.subtract),
              reads=[lres], writes=[lres])
        cx.op("dve", lambda e: e.tensor_scalar(out=sm[:, 5:6], in0=sm[:, 4:5], scalar1=-lam_init, scalar2=None, op0=ALU.add),
              reads=[lres], writes=[lres])
        cx.op("dve", lambda e: e.tensor_scalar(out=sm[:, 6:7], in0=sw[:, 0:1], scalar1=1.0 - lam_init, scalar2=None, op0=ALU.mult),
              reads=[lres], writes=[lres])
        nlam = sm[:, 5:6]
        wsc = sm[:, 6:7]

        vt, vres = self.load_v(0)
        qk = [cx.sb([65, 4, S], BF16, "qkA") for _ in range(2)]
        qkres = [Res("qkA") for _ in range(2)]
        oh = [cx.sb([128, S], BF16, "ohA") for _ in range(2)]
        ohres = [Res("ohA") for _ in range(2)]
        NP = 4
        pt = [cx.sb([128, 512], BF16, "ptA") for _ in range(NP)]
        ptres = [Res("ptA") for _ in range(NP)]
        rcp = cx.sb([128, 512], F32, "rcpA")
        rcpres = Res("rcpA")
        tn = [cx.sb([128, 512], F32, "tnA") for _ in range(2)]
        tnres = [Res("tnA") for _ in range(2)]
        o32 = cx.sb([128, 512], F32, "o32A")
        o32res = Res("o32A")
        rs = cx.sb([128, 512], F32, "rsA")
        rsres = Res("rsA")
        sqb = cx.sb([128, 512], BF16, "sqA")
        sqres = Res("sqA")

        def load_head(h):
            hb = h % 2
            r0 = h * 128
            cx.dma("sp", qk[hb][0:64, 0:2, :], self.projT[r0:r0 + 128, :].rearrange("(m p) s -> p m s", p=64),
                   writes=[qkres[hb]])
            cx.dma("sp", qk[hb][0:64, 2:4, :], self.projT[1024 + r0:1024 + r0 + 128, :].rearrange("(m p) s -> p m s", p=64),
                   writes=[qkres[hb]])
            for m in range(2):
                cx.dma("sp", qk[hb][64:65, m, :], self.c_rows[h:h + 1, :], writes=[qkres[hb]])
                cx.dma("sp", qk[hb][64:65, 2 + m, :], self.c_rows[8:9, :], writes=[qkres[hb]])

        tasks = []
        u = 0
        for h in range(8):
            for c in range(4):
                nkb = 4 * c + 4
                for m in range(2):
                    for kb in range(nkb):
                        tasks.append(dict(h=h, c=c, m=m, kb=kb, nkb=nkb, u=u, first_of_head=(c == 0 and m == 0 and kb == 0),
                                          last_of_head=(c == 3 and m == 1 and kb == nkb - 1)))
                    u += 1
        self.sctr = 0
        load_head(0)

        def s1(t, step):
            h, c, m, kb = t["h"], t["c"], t["m"], t["kb"]
            hb = h % 2
            if t["first_of_head"] and h + 1 < 8:
                load_head(h + 1)
            d = kb - 4 * c
            c0 = 128 * d if d > 0 else 0
            bS = self.sctr % 4
            self.sctr += 1
            S_ = self.ps[bS]
            p_ = t["p"] = bS
            t["c0"] = c0
            cx.op("pe", lambda e: e.matmul(S_[:, c0:512], qk[hb][0:65, 2 + m, kb * 128:(kb + 1) * 128],
                                           qk[hb][0:65, m, c * 512 + c0:(c + 1) * 512], start=True, stop=True),
                  reads=[qkres[hb]], writes=[self.pres[bS]])
            if d >= 0:
                cx.op("dve", lambda e: e.tensor_tensor(
                    out=S_[:, 128 * d:128 * d + 128], in0=S_[:, 128 * d:128 * d + 128],
                    in1=cf[:, CF_TRINEG:CF_TRINEG + 128], op=ALU.add),
                    reads=[self.pres[bS], self.cres], writes=[self.pres[bS]])
            bcol = CF_KPA + h * 16 + kb
            cx.op("act", lambda e: e.activation(out=pt[p_][:, c0:512], in_=S_[:, c0:512], func=AF.Exp,
                                                bias=cf[:, bcol:bcol + 1], scale=1.0),
                  reads=[self.pres[bS], self.cres], writes=[ptres[p_]])

        def s2(t, step):
            h, c, m, kb, nkb, u_ = t["h"], t["c"], t["m"], t["kb"], t["nkb"], t["u"]
            hb = h % 2
            p_, c0 = t["p"], t["c0"]
            bO = 4 + 2 * (u_ % 2)
            bSm = bO + 1

            def mmO(e):
                e.matmul(self.ps[bO][:, c0:512], vt[:, kb, h * 128:(h + 1) * 128], pt[p_][:, c0:512],
                         start=(kb == 0), stop=(kb == nkb - 1))
                return e.matmul(self.ps[bSm][:, c0:512], ones, pt[p_][:, c0:512], start=(kb == 0), stop=(kb == nkb - 1))
            cx.op("pe", mmO, reads=[ptres[p_], vres, self.cres], writes=[self.pres[bO], self.pres[bSm]])
            if kb != nkb - 1:
                return
            def fin0():
                cx.op("act", lambda e: e.activation(out=rcp[:], in_=self.ps[bSm][:], func=AF.Ln), reads=[self.pres[bSm]], writes=[rcpres])
                cx.op("act", lambda e: e.activation(out=rcp[:], in_=rcp[:], func=AF.Exp, scale=-1.0), reads=[rcpres], writes=[rcpres])
                cx.op("dve", lambda e: e.tensor_tensor(out=tn[m][:], in0=self.ps[bO][:], in1=rcp[:], op=ALU.mult),
                      reads=[self.pres[bO], rcpres], writes=[tnres[m]])
            self.defer(step + 1, fin0)
            if m == 0:
                return

            def fin1():
                cx.op("dve", lambda e: e.scalar_tensor_tensor(out=o32[:], in0=tn[1][:], scalar=nlam, in1=tn[0][:],
                                                              op0=ALU.mult, op1=ALU.add),
                      reads=[tnres[0], tnres[1], lres], writes=[o32res])

            def fin2():
                cx.op("act", lambda e: e.activation(out=sqb[:], in_=o32[:], func=AF.Square), reads=[o32res], writes=[sqres])
                bq = self.sctr % 4
                self.sctr += 1
                t["bq"] = bq
                cx.op("pe", lambda e: e.matmul(self.ps[bq][:], ones, sqb[:], start=True, stop=True),
                      reads=[sqres, self.cres], writes=[self.pres[bq]])

            def fin3():
                bq = t["bq"]
                cx.op("act", lambda e: e.activation(out=rs[:], in_=self.ps[bq][:], func=AF.Ln,
                                                    bias=cf[:, CF_EPS:CF_EPS + 1], scale=1.0 / 128),
                      reads=[self.pres[bq], self.cres], writes=[rsres])
                cx.op("act", lambda e: e.activation(out=rs[:], in_=rs[:], func=AF.Exp, scale=-0.5), reads=[rsres], writes=[rsres])
                cx.op("dve", lambda e: e.scalar_tensor_tensor(
                    out=oh[hb][:, c * 512:(c + 1) * 512], in0=o32[:], scalar=wsc, in1=rs[:], op0=ALU.mult, op1=ALU.mult),
                    reads=[o32res, rsres, lres], writes=[ohres[hb]])
                if t["last_of_head"]:
                    cx.dma("sp", self.oT[0, h * 128:(h + 1) * 128, :], oh[hb][:], reads=[ohres[hb]])
            self.defer(step + 2, fin1)
            self.defer(step + 3, fin2)
            self.defer(step + 5, fin3)

        self.run_pipeline(tasks, [s1, s2], [0, 2])

    def phase_attn_b(self, l):
        cx = self.cx
        cx.barrier()
        cx.release(self.persist_mark)
        cf, cb = self.cf, self.cb
        ones = self.ones()
        tri = cb[:, CB_TRI:CB_TRI + 128]
        vt, vres = self.load_v(1)
        qk = [cx.sb([128, 3, S], BF16, "qkB") for _ in range(2)]
        qkres = [Res("qkB") for _ in range(2)]
        oh = [cx.sb([128, S], BF16, "ohB") for _ in range(2)]
        ohres = [Res("ohB") for _ in range(2)]
        NB = 3
        ef = [cx.sb([128, 512], F32, "eB") for _ in range(NB)]
        efres = [Res("eB") for _ in range(NB)]
        lb = [cx.sb([128, 512], BF16, "lB") for _ in range(NB)]
        lbres = [Res("lB") for _ in range(NB)]
        tmp = [cx.sb([128, 512], F32, "tB") for _ in range(2)]
        tmpres = [Res("tB") for _ in range(2)]
        pt = [cx.sb([128, 512], BF16, "ptB") for _ in range(NB)]
        ptres = [Res("ptB") for _ in range(NB)]
        cs = cx.sb([128, 512], F32, "csB")
        csres = Res("csB")

        def load_head(h):
            hb = h % 2
            r0 = 3072 + h * 128
            cx.dma("sp", qk[hb][:, 0, :], self.projT[r0:r0 + 128, :], writes=[qkres[hb]])
            cx.dma("sp", qk[hb][:, 1, :], self.projT[r0 + 1024:r0 + 1024 + 128, :], writes=[qkres[hb]])
            cx.op("dve", lambda e: e.tensor_scalar(out=qk[hb][:, 2, :], in0=qk[hb][:, 1, :], scalar1=-1.0,
                                                   scalar2=None, op0=ALU.mult),
                  reads=[qkres[hb]], writes=[qkres[hb]])

        tasks = []
        u = 0
        i = 0
        for h in range(8):
            for c in range(4):
                nkb = 4 * c + 4
                for kb in range(nkb - 1, -1, -1):
                    tasks.append(dict(h=h, c=c, kb=kb, nkb=nkb, u=u, i=i, first_of_head=(c == 0 and kb == nkb - 1),
                                      last_of_head=(c == 3 and kb == 0)))
                    i += 1
                u += 1
        load_head(0)

        def s1(t, step):
            h, c, kb, i = t["h"], t["c"], t["kb"], t["i"]
            hb = h % 2
            if t["first_of_head"] and h + 1 < 8:
                load_head(h + 1)
            d = kb - 4 * c
            c0 = t["c0"] = 128 * d if d > 0 else 0
            j = i % NB
            bZ = i % 2
            Z = self.ps[bZ]
            qs = slice(c * 512 + c0, (c + 1) * 512)
            ks = slice(kb * 128, (kb + 1) * 128)
            cx.op("pe", lambda e: e.matmul(Z[:, c0:512], qk[hb][:, 1, ks], qk[hb][:, 0, qs], start=True, stop=True),
                  reads=[qkres[hb]], writes=[self.pres[bZ]])
            cx.op("act", lambda e: e.activation(out=ef[j][:, c0:512], in_=Z[:, c0:512], func=AF.Exp),
                  reads=[self.pres[bZ]], writes=[efres[j]])
            cx.op("act", lambda e: e.activation(out=lb[j][:, c0:512], in_=ef[j][:, c0:512], func=AF.Ln,
                                                bias=cf[:, CF_ONE:CF_ONE + 1], scale=1.0),
                  reads=[efres[j], self.cres], writes=[lbres[j]])
            if d >= 0:
                cx.op("dve", lambda e: e.tensor_tensor(
                    out=lb[j][:, 128 * d:128 * d + 128], in0=lb[j][:, 128 * d:128 * d + 128],
                    in1=cb[:, CB_M01S:CB_M01S + 128], op=ALU.mult),
                    reads=[lbres[j], self.cres], writes=[lbres[j]])

        def s2(t, step):
            h, c, kb, nkb, i = t["h"], t["c"], t["kb"], t["nkb"], t["i"]
            hb = h % 2
            c0 = t["c0"]
            d = kb - 4 * c
            j = i % NB
            j2 = i % 2
            bX, bC = 2 + (i % 2), 4 + (i % 2)
            X, CBk = self.ps[bX], self.ps[bC]
            qs = slice(c * 512 + c0, (c + 1) * 512)
            ks = slice(kb * 128, (kb + 1) * 128)
            if kb == nkb - 1:
                cx.op("dve", lambda e: e.memset(cs[:], 0.0), writes=[csres])

            def mmX(e):
                e.matmul(X[:, c0:512], tri, lb[j][:, c0:512], start=True, stop=False)
                e.matmul(X[:, c0:512], qk[hb][:, 2, ks], qk[hb][:, 0, qs], start=False, stop=True)
                return e.matmul(CBk[:, c0:512], ones, lb[j][:, c0:512], start=True, stop=True)
            cx.op("pe", mmX, reads=[lbres[j], qkres[hb], self.cres], writes=[self.pres[bX], self.pres[bC]])
            cx.op("dve", lambda e: e.tensor_tensor(out=tmp[j2][:, c0:512], in0=X[:, c0:512], in1=cs[:, c0:512], op=ALU.add),
                  reads=[self.pres[bX], csres], writes=[tmpres[j2]])
            if d >= 0:
                cx.op("dve", lambda e: e.tensor_tensor(
                    out=tmp[j2][:, 128 * d:128 * d + 128], in0=tmp[j2][:, 128 * d:128 * d + 128],
                    in1=cf[:, CF_TRIPOS:CF_TRIPOS + 128], op=ALU.add),
                    reads=[tmpres[j2], self.cres], writes=[tmpres[j2]])
            if kb > 0:
                cx.op("dve", lambda e: e.tensor_tensor(out=cs[:, c0:512], in0=CBk[:, c0:512], in1=cs[:, c0:512], op=ALU.add),
                      reads=[self.pres[bC], csres], writes=[csres])

        def s2b(t, step):
            i = t["i"]
            c0 = t["c0"]
            j = i % NB
            j2 = i % 2
            cx.op("act", lambda e: e.activation(out=pt[j][:, c0:512], in_=tmp[j2][:, c0:512], func=AF.Exp, scale=-1.0),
                  reads=[tmpres[j2]], writes=[ptres[j]])
            if t["kb"] == t["nkb"] - 1 and c0 > 0:
                cx.op("dve", lambda e: e.memset(pt[j][:, 0:c0], 0.0), writes=[ptres[j]])

        def s3(t, step):
            h, c, kb, nkb, i, u_ = t["h"], t["c"], t["kb"], t["nkb"], t["i"], t["u"]
            hb = h % 2
            c0 = t["c0"]
            j = i % NB
            bO = 6 + (u_ % 2)
            cO = 0 if kb == nkb - 1 else c0
            cx.op("pe", lambda e: e.matmul(self.ps[bO][:, cO:512], vt[:, kb, h * 128:(h + 1) * 128], pt[j][:, cO:512],
                                           start=(kb == nkb - 1), stop=(kb == 0)),
                  reads=[ptres[j], vres], writes=[self.pres[bO]])
            if kb == 0:
                def fin():
                    cx.op("act", lambda e: e.activation(out=oh[hb][:, c * 512:(c + 1) * 512], in_=self.ps[bO][:], func=AF.Copy),
                          reads=[self.pres[bO]], writes=[ohres[hb]])
                    if t["last_of_head"]:
                        cx.dma("sp", self.oT[1, h * 128:(h + 1) * 128, :], oh[hb][:], reads=[ohres[hb]])
                self.defer(step + 1, fin)

        self.run_pipeline(tasks, [s1, s2, s2b, s3], [0, 2, 3, 5])

    def phase_attn_c(self, l):
        cx = self.cx
        cx.barrier()
        cx.release(self.persist_mark)
        cf, cb = self.cf, self.cb
        ones = self.ones()
        ident = cb[:, CB_IDENT:CB_IDENT + 128]
        vt, vres = self.load_v(2)
        ltab = cx.sb([9, S], BF16, "ltab")
        ltres = Res("ltab")
        cx.dma("sp", ltab[:], self.c_rows[17:26, :], writes=[ltres])
        qk = [cx.sb([128, 2, S], BF16, "qkC") for _ in range(2)]
        qkres = [Res("qkC") for _ in range(2)]
        rt = [cx.sb([9, S], BF16, "rtC") for _ in range(2)]
        rtres = [Res("rtC") for _ in range(2)]
        oh = [cx.sb([128, S], BF16, "ohC") for _ in range(2)]
        ohres = [Res("ohC") for _ in range(2)]
        NP = 3
        pt = [cx.sb([128, 512], BF16, "ptC") for _ in range(NP)]
        ptres = [Res("ptC") for _ in range(NP)]
        km = cx.sb([128, 8], F32, "km")
        kh = cx.sb([128, 8], BF16, "kh")
        kl = cx.sb([128, 8], BF16, "kl")
        gm = cx.sb([128, 128], F32, "gm")
        mx = cx.sb([128, 128], F32, "mx")
        sel = cx.sb([128, 128], F32, "sel")
        mb = cx.sb([128, 128], BF16, "mb")
        gres = Res("gsel")
        rr = cx.sb([128, 512], F32, "rrC")
        rrres = Res("rrC")

        def load_head(h):
            hb = h % 2
            r0 = 6144 + h * 128
            cx.dma("sp", qk[hb][:, 0, :], self.projT[r0:r0 + 128, :], writes=[qkres[hb]])
            cx.dma("sp", qk[hb][:, 1, :], self.projT[r0 + 1024:r0 + 1024 + 128, :], writes=[qkres[hb]])
            cx.dma("sp", rt[hb][8:9, :], self.c_rows[9 + h:10 + h, :], writes=[rtres[hb]])

        def gate_head(h, gstep=None):
            hb = h % 2
            cx.op("dve", lambda e: e.tensor_reduce(out=km[:], in_=qk[hb][:, 1, :].rearrange("p (n k) -> p n k", k=256),
                                                   axis=AX.X, op=ALU.add),
                  reads=[qkres[hb]], writes=[gres])
            cx.op("dve", lambda e: e.tensor_copy(out=kh[:], in_=km[:]), reads=[gres], writes=[gres])
            cx.op("dve", lambda e: e.tensor_tensor(out=kl[:], in0=km[:], in1=kh[:], op=ALU.subtract), reads=[gres], writes=[gres])
            bG = 6

            def mmG(e):
                ins = None
                for qb in range(16):
                    e.matmul(self.ps[bG][:, qb * 8:(qb + 1) * 8], qk[hb][:, 0, qb * 128:(qb + 1) * 128], kh[:], start=True, stop=False)
                    ins = e.matmul(self.ps[bG][:, qb * 8:(qb + 1) * 8], qk[hb][:, 0, qb * 128:(qb + 1) * 128], kl[:], start=False, stop=True)
                return ins
            cx.op("pe", mmG, reads=[qkres[hb], gres], writes=[self.pres[bG]])
            cx.op("dve", lambda e: e.tensor_tensor(out=gm[:], in0=self.ps[bG][:, 0:128], in1=cf[:, CF_PAD:CF_PAD + 128], op=ALU.add),
                  reads=[self.pres[bG], self.cres], writes=[gres])
            for qb in range(16):
                cx.op("dve", lambda e, qb=qb: e.max(out=mx[:, qb * 8:(qb + 1) * 8], in_=gm[:, qb * 8:(qb + 1) * 8]),
                      reads=[gres], writes=[gres])
            for qb in range(16):
                cx.op("dve", lambda e, qb=qb: e.tensor_scalar(
                    out=sel[:, qb * 8:(qb + 1) * 8], in0=gm[:, qb * 8:(qb + 1) * 8], scalar1=mx[:, qb * 8 + 2:qb * 8 + 3],
                    scalar2=-NEG, op0=ALU.is_ge, op1=ALU.mult), reads=[gres], writes=[gres])
            cx.op("dve", lambda e: e.scalar_tensor_tensor(out=mb[:], in0=sel[:], scalar=NEG, in1=cf[:, CF_PAST:CF_PAST + 128],
                                                          op0=ALU.add, op1=ALU.mult),
                  reads=[gres, self.cres], writes=[gres])
            def transposes():
              for half in range(2):
                bT = 6 + half
                tb = self.ps[bT][:, :].bitcast(BF16)

                def mmT(e, half=half, tb=tb):
                    ins = None
                    for q8 in range(8):
                        qb = half * 8 + q8
                        ins = e.transpose(tb[0:8, q8 * 128:(q8 + 1) * 128], mb[:, qb * 8:(qb + 1) * 8], ident)
                    return ins
                cx.op("pe", mmT, reads=[gres, self.cres], writes=[self.pres[bT]])
                cx.op("act", lambda e, half=half, tb=tb: e.activation(
                    out=rt[hb][0:8, half * 1024:(half + 1) * 1024], in_=tb[0:8, :], func=AF.Copy),
                    reads=[self.pres[bT]], writes=[rtres[hb]])
            if gstep is None:
                transposes()
            else:
                self.defer(gstep + 14, transposes)

        tasks = []
        u = 0
        i = 0
        for h in range(8):
            for c in range(4):
                nkb = 4 * c + 4
                for kb in range(nkb):
                    tasks.append(dict(h=h, c=c, kb=kb, nkb=nkb, u=u, i=i, first_of_head=(c == 0 and kb == 0),
                                      mid_of_head=(c == 2 and kb == 0), last_of_head=(c == 3 and kb == nkb - 1)))
                    i += 1
                u += 1
        load_head(0)
        gate_head(0)

        def s1(t, step):
            h, c, kb, i = t["h"], t["c"], t["kb"], t["i"]
            hb = h % 2
            if t["first_of_head"] and h + 1 < 8:
                load_head(h + 1)
            if t["mid_of_head"] and h + 1 < 8:
                gate_head(h + 1, step)
            d = kb - 4 * c
            c0 = t["c0"] = 128 * d if d > 0 else 0
            bS = i % 2
            S_ = self.ps[bS]
            p_ = i % NP
            qs = slice(c * 512 + c0, (c + 1) * 512)
            ks = slice(kb * 128, (kb + 1) * 128)

            def mmS(e):
                e.matmul(S_[:, c0:512], qk[hb][:, 1, ks], qk[hb][:, 0, qs], start=True, stop=False)
                return e.matmul(S_[:, c0:512], ltab[0:9, ks], rt[hb][0:9, qs], start=False, stop=True)
            cx.op("pe", mmS, reads=[qkres[hb], rtres[hb], ltres], writes=[self.pres[bS]])
            if d >= 0:
                cx.op("dve", lambda e: e.tensor_tensor(
                    out=S_[:, 128 * d:128 * d + 128], in0=S_[:, 128 * d:128 * d + 128],
                    in1=cf[:, CF_TRINEG:CF_TRINEG + 128], op=ALU.add),
                    reads=[self.pres[bS], self.cres], writes=[self.pres[bS]])
            bcol = CF_KPC + h * 16 + kb
            cx.op("act", lambda e: e.activation(out=pt[p_][:, c0:512], in_=S_[:, c0:512], func=AF.Exp,
                                                bias=cf[:, bcol:bcol + 1], scale=1.0),
                  reads=[self.pres[bS], self.cres], writes=[ptres[p_]])

        def s2(t, step):
            h, c, kb, nkb, i, u_ = t["h"], t["c"], t["kb"], t["nkb"], t["i"], t["u"]
            hb = h % 2
            c0 = t["c0"]
            p_ = i % NP
            bO = 2 + 2 * (u_ % 2)
            bSm = bO + 1

            def mmO(e):
                e.matmul(self.ps[bO][:, c0:512], vt[:, kb, h * 128:(h + 1) * 128], pt[p_][:, c0:512],
                         start=(kb == 0), stop=(kb == nkb - 1))
                return e.matmul(self.ps[bSm][:, c0:512], ones, pt[p_][:, c0:512], start=(kb == 0), stop=(kb == nkb - 1))
            cx.op("pe", mmO, reads=[ptres[p_], vres, self.cres], writes=[self.pres[bO], self.pres[bSm]])
            if kb == nkb - 1:
                def fin():
                    cx.op("act", lambda e: e.activation(out=rr[:], in_=self.ps[bSm][:], func=AF.Ln), reads=[self.pres[bSm]], writes=[rrres])
                    cx.op("act", lambda e: e.activation(out=rr[:], in_=rr[:], func=AF.Exp, scale=-1.0), reads=[rrres], writes=[rrres])
                    cx.op("dve", lambda e: e.tensor_tensor(out=oh[hb][:, c * 512:(c + 1) * 512], in0=self.ps[bO][:], in1=rr[:],
                                                           op=ALU.mult),
                          reads=[self.pres[bO], rrres], writes=[ohres[hb]])
                    if t["last_of_head"]:
                        cx.dma("sp", self.oT[2, h * 128:(h + 1) * 128, :], oh[hb][:], reads=[ohres[hb]])
                self.defer(step + 1, fin)

        self.run_pipeline(tasks, [s1, s2], [0, 1])

    def phase_merge(self, l):
        cx = self.cx
        cx.barrier()
        cx.release(self.persist_mark)
        oall = cx.sb([128, 24, S], BF16, "oall")
        ores = [Res("oall%d" % i) for i in range(3)]
        for i in range(3):
            cx.dma("pool", oall[:, i * 8:(i + 1) * 8, :], self.oT[i].rearrange("(hc p) s -> p hc s", p=128), writes=[ores[i]], phase_local=True)
        gsb = [cx.sb([128, 3, S], BF16, "gsb") for _ in range(2)]
        gsres = [Res("gsb") for _ in range(2)]
        mst = [cx.sb([128, S], BF16, "mst") for _ in range(2)]
        mstres = [Res("mst") for _ in range(2)]
        ta = cx.sb([128, 512], F32, "ta")
        tbb = cx.sb([128, 512], F32, "tb")
        tares, tbres = Res("ta"), Res("tb")
        wv = self.w_branch[l].rearrange("i (hc p) n -> p i hc n", p=128)
        bi = 0
        for cg in range(4):
            c0 = cg * 512
            s = self.next_slot()
            slot = self.wslot[s][:, 0:12288].rearrange("p (i hc n) -> p i hc n", i=3, hc=8)
            for i in range(3):
                cx.dma("pool", slot[:, i, :, :], wv[:, i, :, c0:c0 + 512], writes=[self.wres[s]])
            for sub in range(4):
                b = bi % 2
                bi += 1
                dblk = cg * 4 + sub
                for i in range(3):
                    cx.dma("sp", gsb[b][:, i, :], self.gateT[i * 2048 + dblk * 128:i * 2048 + (dblk + 1) * 128, :],
                           writes=[gsres[b]])
                for tg in range(4):
                    zb = []
                    for i in range(3):
                        pst, pr = self.bank()
                        zb.append((pst, pr))

                        def mm(e, i=i, pst=pst, slot=slot, sub=sub, tg=tg):
                            ins = None
                            for hc in range(8):
                                ins = e.matmul(pst[:], slot[:, i, hc, sub * 128:(sub + 1) * 128],
                                               oall[:, i * 8 + hc, tg * 512:(tg + 1) * 512], start=(hc == 0), stop=(hc == 7))
                            return ins
                        cx.op("pe", mm, reads=[self.wres[s], ores[i]], writes=[pr])
                    ts = slice(tg * 512, (tg + 1) * 512)
                    cx.op("dve", lambda e, b=b, ts=ts, z=zb[0][0]: e.tensor_tensor(out=ta[:], in0=z[:], in1=gsb[b][:, 0, ts], op=ALU.mult),
                          reads=[zb[0][1], gsres[b]], writes=[tares])
                    cx.op("dve", lambda e, b=b, ts=ts, z=zb[1][0]: e.tensor_tensor(out=tbb[:], in0=z[:], in1=gsb[b][:, 1, ts], op=ALU.mult),
                          reads=[zb[1][1], gsres[b]], writes=[tbres])
                    cx.op("dve", lambda e: e.tensor_tensor(out=ta[:], in0=ta[:], in1=tbb[:], op=ALU.add),
                          reads=[tares, tbres], writes=[tares])
                    cx.op("dve", lambda e, b=b, ts=ts, z=zb[2][0]: e.tensor_tensor(out=tbb[:], in0=z[:], in1=gsb[b][:, 2, ts], op=ALU.mult),
                          reads=[zb[2][1], gsres[b]], writes=[tbres])
                    cx.op("dve", lambda e, b=b, ts=ts: e.tensor_tensor(out=mst[b][:, ts], in0=ta[:], in1=tbb[:], op=ALU.add),
                          reads=[tares, tbres], writes=[mstres[b]])
                cx.dma("sp", self.mergedT[dblk * 128:(dblk + 1) * 128, :], mst[b][:], reads=[mstres[b]])

    def residual_gemm(self, xsrc, w_ap, a_dram, nkc, npass=1):
        cx = self.cx
        cx.barrier()
        cx.release(self.persist_mark)
        nb = 1 if npass == 1 else 2
        aT = [cx.sb([128, nkc, S], BF16, "aT") for _ in range(nb)]
        ares = [Res("aT") for _ in range(nb)]
        xr = [cx.sb([128, S], F32, "xr") for _ in range(3)]
        xrres = [Res("xr") for _ in range(3)]
        rows = nkc * 128

        def load_a(p):
            cx.dma("pool", aT[p % nb][:], a_dram[p * rows:(p + 1) * rows, :].rearrange("(kc p) s -> p kc s", p=128),
                   writes=[ares[p % nb]], phase_local=(p < 2))
        load_a(0)
        xi = 0
        for p in range(npass):
            wv = w_ap[p * rows:(p + 1) * rows, :].rearrange("(kc p) n -> p kc n", p=128)
            src_x = xsrc if p == 0 else self.xres
            at = aT[p % nb]
            for cg in range(4):
                c0 = cg * 512
                s = self.next_slot()
                slot = self.wslot[s][:, 0:nkc * 512].rearrange("p (kc n) -> p kc n", kc=nkc)
                cx.dma("pool", slot, wv[:, :, c0:c0 + 512], writes=[self.wres[s]])
                if cg == 0 and p + 1 < npass:
                    load_a(p + 1)
                for sub in range(4):
                    blk = cg * 4 + sub
                    b = xi % 3
                    xi += 1
                    cx.dma("sp", xr[b][:], src_x[blk * 128:(blk + 1) * 128, :], reads=[self.xres_r[blk]], writes=[xrres[b]])
                    for tg in range(4):
                        pst, pr = self.bank()

                        def mm(e, pst=pst, slot=slot, sub=sub, tg=tg, at=at):
                            ins = None
                            for kc in range(nkc):
                                ins = e.matmul(pst[:], slot[:, kc, sub * 128:(sub + 1) * 128], at[:, kc, tg * 512:(tg + 1) * 512],
                                               start=(kc == 0), stop=(kc == nkc - 1))
                            return ins
                        cx.op("pe", mm, reads=[self.wres[s], ares[p % nb]], writes=[pr])
                        ts = slice(tg * 512, (tg + 1) * 512)
                        cx.op("dve", lambda e, b=b, ts=ts, pst=pst: e.tensor_tensor(out=xr[b][:, ts], in0=pst[:], in1=xr[b][:, ts], op=ALU.add),
                              reads=[pr, xrres[b]], writes=[xrres[b]])
                    cx.dma("sp", self.xres[blk * 128:(blk + 1) * 128, :], xr[b][:], reads=[xrres[b]], writes=[self.xres_r[blk]])

    def phase_ffn_up(self, l):
        cx = self.cx
        cx.barrier()
        cx.release(self.persist_mark)
        hT = cx.sb([128, 16, S], BF16, "hT2")
        hres = [Res("hT2_%d" % i) for i in range(16)]
        m = self.rmsnorm_to_sbuf(self.xres, self.g_ffn[l], hT, hres)
        cx.release(m)
        ast = [cx.sb([128, S], BF16, "ast") for _ in range(2)]
        astres = [Res("ast") for _ in range(2)]
        sg = [cx.sb([128, 512], F32, "sg") for _ in range(2)]
        sgres = [Res("sg") for _ in range(2)]
        wg = self.w_gate[l].rearrange("(kc p) n -> p kc n", p=128)
        wu = self.w_up[l].rearrange("(kc p) n -> p kc n", p=128)
        ai = 0
        si = 0
        for fg in range(22):
            f0 = fg * 256
            s = self.next_slot()
            slot = self.wslot[s][:, 0:8192].rearrange("p (g kc n) -> p g kc n", g=2, kc=16)
            cx.dma("pool", slot[:, 0, :, :], wg[:, :, f0:f0 + 256], writes=[self.wres[s]])
            cx.dma("pool", slot[:, 1, :, :], wu[:, :, f0:f0 + 256], writes=[self.wres[s]])
            for sub in range(2):
                b = ai % 2
                ai += 1
                fblk = fg * 2 + sub
                for tg in range(4):
                    pg, prg = self.bank()
                    pu, pru = self.bank()

                    def mm(e, pg=pg, pu=pu, slot=slot, sub=sub, tg=tg):
                        ins = None
                        for kc in range(16):
                            e.matmul(pg[:], slot[:, 0, kc, sub * 128:(sub + 1) * 128], hT[:, kc, tg * 512:(tg + 1) * 512],
                                     start=(kc == 0), stop=(kc == 15))
                        for kc in range(16):
                            ins = e.matmul(pu[:], slot[:, 1, kc, sub * 128:(sub + 1) * 128], hT[:, kc, tg * 512:(tg + 1) * 512],
                                           start=(kc == 0), stop=(kc == 15))
                        return ins
                    cx.op("pe", mm, reads=[self.wres[s]] + hres, writes=[prg, pru])
                    j = si % 2
                    si += 1
                    cx.op("act", lambda e, j=j, pg=pg: e.activation(out=sg[j][:], in_=pg[:], func=AF.Silu),
                          reads=[prg], writes=[sgres[j]])
                    ts = slice(tg * 512, (tg + 1) * 512)
                    cx.op("dve", lambda e, j=j, b=b, ts=ts, pu=pu: e.tensor_tensor(out=ast[b][:, ts], in0=pu[:], in1=sg[j][:], op=ALU.mult),
                          reads=[pru, sgres[j]], writes=[astres[b]])
                cx.dma("sp", self.actT[fblk * 128:(fblk + 1) * 128, :], ast[b][:], reads=[astres[b]])

    def phase_final(self, xsrc):
        cx = self.cx
        cx.barrier()
        cx.release(self.persist_mark)
        self.rmsnorm_to_sbuf(xsrc, self.g_fin, None, None, out_dram=self.outT)


CB_ONES = 0
CB_TRI = 128
CB_IDENT = 256
CB_M01S = 384
CB_COLS = 512
CF_EPS = 0
CF_ONE = 1
CF_KPA = 8
CF_KPC = 136
CF_TRINEG = 264
CF_TRIPOS = 392
CF_PAD = 520
CF_PAST = 648
CF_COLS = 776
NROWS = 26


def make_consts():
    p = np.arange(128)
    cb = np.zeros((128, CB_COLS), dtype=np.float32)
    cb[:, CB_ONES:CB_ONES + 128] = 1.0
    cb[:, CB_TRI:CB_TRI + 128] = (p[:, None] >= p[None, :])
    cb[:, CB_IDENT:CB_IDENT + 128] = np.eye(128)
    cb[:, CB_M01S:CB_M01S + 128] = (p[:, None] < p[None, :])
    cf = np.zeros((128, CF_COLS), dtype=np.float32)
    cf[:, CF_EPS] = EPS
    cf[:, CF_ONE] = 1.0
    for h in range(8):
        for kb in range(16):
            cf[:, CF_KPA + h * 16 + kb] = ALIBI_DIFF[h] * (kb * 128 + p)
            cf[:, CF_KPC + h * 16 + kb] = ALIBI_MOBA[h] * (kb * 128 + p)
    cf[:, CF_TRINEG:CF_TRINEG + 128] = np.where(p[:, None] <= p[None, :], 0.0, NEG)
    cf[:, CF_TRIPOS:CF_TRIPOS + 128] = np.where(p[:, None] < p[None, :], 0.0, -NEG)
    for qb in range(16):
        own = qb // 2
        for n in range(8):
            cf[:, CF_PAD + qb * 8 + n] = 0.0 if n < own else -3.0e38
            cf[:, CF_PAST + qb * 8 + n] = 1.0 if n < own else 0.0
    t = np.arange(S, dtype=np.float64)
    rows = np.zeros((NROWS, S), dtype=np.float32)
    for h in range(8):
        rows[h] = -ALIBI_DIFF[h] * t
        rows[9 + h] = -ALIBI_MOBA[h] * t
    rows[8] = 1.0
    for n in range(8):
        rows[17 + n] = (np.arange(S) // 256 == n)
    rows[25] = 1.0
    return cb.astype(ml_dtypes.bfloat16), cf, rows.astype(ml_dtypes.bfloat16)


def make_inputs(inp, b):
    cb, cf, rows = make_consts()
    f = lambda a: np.ascontiguousarray(a, dtype=np.float32)
    d = {
        "xT": f(inp["x"][b].T),
        "w_in": f(inp["w_in"]), "w_branch": f(inp["w_branch"]), "w_out": f(inp["w_out"]),
        "w_ffn_gate": f(inp["w_ffn_gate"]), "w_ffn_up": f(inp["w_ffn_up"]), "w_ffn_down": f(inp["w_ffn_down"]),
        "g_mix": f(inp["norm_mix_g"].reshape(DEPTH, 16, 128).transpose(0, 2, 1)),
        "g_ffn": f(inp["norm_ffn_g"].reshape(DEPTH, 16, 128).transpose(0, 2, 1)),
        "g_fin": f(inp["final_norm_g"].reshape(16, 128).T),
        "b_gate": f(inp["b_gate"].reshape(DEPTH, 48, 128).transpose(0, 2, 1)),
        "lamv": f(np.stack([inp["lam_q1"], inp["lam_k1"], inp["lam_q2"], inp["lam_k2"]], axis=1)),
        "subln": f(inp["subln_w"].reshape(DEPTH, 128, 1)),
        "c_bf": cb, "c_f32": cf, "c_rows": rows,
    }
    return d


def kernel(**inputs):
    prog = Prog()
    n = 8
    shared = None
    in_maps = []
    for b in range(n):
        d = make_inputs(inputs, b) if shared is None else dict(shared, xT=np.ascontiguousarray(inputs["x"][b].T, dtype=np.float32))
        if shared is None:
            shared = d
        in_maps.append(d)
    res = run_bass_kernel_spmd(prog.nc, in_maps, core_ids=list(range(n)))
    out = np.stack([np.ascontiguousarray(r["outT"].T) for r in res.results], axis=0)
    return out.astype(np.float32)
```
